# Optimizing a Trainium2 kernel written in Bass

```python
import math
import jax, jax.numpy as jnp
from jax import lax
import numpy as np

D_MODEL = 2048
BATCH = 2
SEQ = 8192
DEPTH = 1

HEAD_DIM = 64
N_Q_HEADS = 16
N_KV_HEADS = 2
ATTN_WIDTH = N_Q_HEADS * HEAD_DIM
KV_WIDTH = N_KV_HEADS * HEAD_DIM
CONV_WIDTH = D_MODEL - ATTN_WIDTH
CONV_SIZE = 31
WINDOW = 128
Q_BLOCK = 128
ROPE_THETA = 10000.0
IN_WIDTH = ATTN_WIDTH + 2 * KV_WIDTH + 2 * CONV_WIDTH
N_GROUPS = 8
EXPERTS_PER_GROUP = 8
N_EXPERTS = N_GROUPS * EXPERTS_PER_GROUP
TOP_K = 2
EXPERT_FF = 512
MOE_BLOCK = 128
RMS_EPS = 1e-6
LN_EPS = 1e-5
NEG_INF = -1e30

kernel_name = "hymba_conformer_swa_sink_hmoe_adaln"


def rmsnorm(x, g):
    xf = x.astype(jnp.float32)
    y = xf * lax.rsqrt(jnp.mean(xf * xf, axis=-1, keepdims=True) + RMS_EPS)
    return (y * g.astype(jnp.float32)).astype(x.dtype)


def layernorm(x, g, b):
    xf = x.astype(jnp.float32)
    mu = jnp.mean(xf, axis=-1, keepdims=True)
    var = jnp.mean(jnp.square(xf - mu), axis=-1, keepdims=True)
    y = (xf - mu) * lax.rsqrt(var + LN_EPS)
    return (y * g.astype(jnp.float32) + b.astype(jnp.float32)).astype(x.dtype)


def modulate(h, shift, scale):
    return h * (1.0 + scale[:, None, :]) + shift[:, None, :]


def apply_rope(t, positions):
    half = HEAD_DIM // 2
    inv_freq = ROPE_THETA ** (-jnp.arange(half, dtype=jnp.float32) * 2.0 / HEAD_DIM)
    ang = positions.astype(jnp.float32)[..., None] * inv_freq
    cos = jnp.cos(ang)[:, :, None, :]
    sin = jnp.sin(ang)[:, :, None, :]
    tf = t.astype(jnp.float32)
    t1, t2 = tf[..., :half], tf[..., half:]
    out = jnp.concatenate([t1 * cos - t2 * sin, t2 * cos + t1 * sin], axis=-1)
    return out.astype(t.dtype)


def sliding_window_sink_attention(q, k, v, sinks):
    B, S, Hq, dh = q.shape
    G = Hq // N_KV_HEADS
    nb = S // Q_BLOCK
    qb = q.reshape(B, nb, Q_BLOCK, N_KV_HEADS, G, dh)
    kb = k.reshape(B, nb, Q_BLOCK, N_KV_HEADS, dh)
    vb = v.reshape(B, nb, Q_BLOCK, N_KV_HEADS, dh)
    pad = ((0, 0), (1, 0), (0, 0), (0, 0), (0, 0))
    kk = jnp.concatenate([jnp.pad(kb, pad)[:, :-1], kb], axis=2)
    vv = jnp.concatenate([jnp.pad(vb, pad)[:, :-1], vb], axis=2)
    s = jnp.einsum('bnqhgd,bnkhd->bnhgqk', qb, kk,
                   preferred_element_type=jnp.float32) * (1.0 / math.sqrt(dh))
    blk = jnp.arange(nb)[:, None]
    qpos = blk * Q_BLOCK + jnp.arange(Q_BLOCK)[None, :]
    kpos = (blk - 1) * Q_BLOCK + jnp.arange(2 * Q_BLOCK)[None, :]
    rel = qpos[:, :, None] - kpos[:, None, :]
    valid = (rel >= 0) & (rel < WINDOW) & (kpos[:, None, :] >= 0)
    s = jnp.where(valid[None, :, None, None], s, NEG_INF)
    sink = sinks.astype(jnp.float32).reshape(N_KV_HEADS, G)[None, None, :, :, None, None]
    m = jnp.maximum(jnp.max(s, axis=-1, keepdims=True), sink)
    p = jnp.exp(s - m)
    p = p / (jnp.sum(p, axis=-1, keepdims=True) + jnp.exp(sink - m))
    o = jnp.einsum('bnhgqk,bnkhd->bnqhgd', p.astype(v.dtype), vv)
    return o.reshape(B, S, Hq * dh)


def conformer_conv(a, b, conv_w, conv_b, ln_g, ln_b):
    u = a * jax.nn.sigmoid(b)
    C = u.shape[-1]
    u = jnp.pad(u, ((0, 0), (CONV_SIZE - 1, 0), (0, 0)))
    y = lax.conv_general_dilated(u, conv_w[:, None, :].astype(u.dtype), window_strides=(1,),
                                 padding='VALID', dimension_numbers=('NWC', 'WIO', 'NWC'),
                                 feature_group_count=C)
    y = y + conv_b
    y = layernorm(y, ln_g, ln_b)
    return jax.nn.silu(y)


def hierarchical_moe(h, w_group_router, b_group_router, w_expert_router, b_expert_router,
                     w_gate_up, w_down):
    B, S, D = h.shape
    T = B * S
    hf = h.reshape(T, D)
    hf32 = hf.astype(jnp.float32)
    g_logits = hf32 @ w_group_router.astype(jnp.float32) + b_group_router.astype(jnp.float32)
    g_prob = jax.nn.softmax(g_logits, axis=-1)
    g_sel = jnp.argmax(g_logits, axis=-1)
    p_g = jnp.take_along_axis(g_prob, g_sel[:, None], axis=1)
    e_logits = (hf32 @ w_expert_router.astype(jnp.float32)
                + b_expert_router.astype(jnp.float32)).reshape(T, N_GROUPS, EXPERTS_PER_GROUP)
    e_in_group = jnp.take_along_axis(e_logits, g_sel[:, None, None], axis=1)[:, 0]
    top_vals, top_loc = lax.top_k(e_in_group, TOP_K)
    weights = p_g * jax.nn.softmax(top_vals, axis=-1)
    expert_ids = g_sel[:, None] * EXPERTS_PER_GROUP + top_loc

    A = T * TOP_K
    flat_e = expert_ids.reshape(A).astype(jnp.int32)
    flat_tok = jnp.repeat(jnp.arange(T, dtype=jnp.int32), TOP_K)
    flat_w = weights.reshape(A)
    order = jnp.argsort(flat_e, stable=True)
    se, stok, sw = flat_e[order], flat_tok[order], flat_w[order]
    counts = jax.ops.segment_sum(jnp.ones((A,), jnp.int32), flat_e, num_segments=N_EXPERTS)
    padded = ((counts + MOE_BLOCK - 1) // MOE_BLOCK) * MOE_BLOCK
    pad_end = jnp.cumsum(padded)
    pad_start = pad_end - padded
    raw_start = jnp.cumsum(counts) - counts
    dest = pad_start[se] + (jnp.arange(A, dtype=jnp.int32) - raw_start[se])
    n_blocks = -(-(A + N_EXPERTS * (MOE_BLOCK - 1)) // MOE_BLOCK)
    rows = n_blocks * MOE_BLOCK
    row_tok = jnp.full((rows,), T, jnp.int32).at[dest].set(stok)
    row_w = jnp.zeros((rows,), flat_w.dtype).at[dest].set(sw)
    block_e = jnp.minimum(jnp.searchsorted(pad_end, jnp.arange(n_blocks) * MOE_BLOCK, side='right'),
                          N_EXPERTS - 1).astype(jnp.int32)
    x_ext = jnp.concatenate([hf, jnp.zeros((1, D), hf.dtype)], axis=0)
    xin = x_ext[row_tok].reshape(n_blocks, MOE_BLOCK, D)

    def expert_block(args):
        xb, e = args
        gu = xb @ w_gate_up[e]
        gate, up = gu[:, :EXPERT_FF], gu[:, EXPERT_FF:]
        return (jax.nn.silu(gate) * up) @ w_down[e]

    ys = lax.map(expert_block, (xin, block_e)).reshape(rows, D)
    out = jnp.zeros((T + 1, D), ys.dtype).at[row_tok].add(ys * row_w[:, None].astype(ys.dtype))
    return out[:T].reshape(B, S, D)


def setup_inputs(seed: int = 0) -> dict:
    key = jax.random.key(seed)
    ks = jax.random.split(key, 24)
    D = D_MODEL
    f32 = jnp.float32
    nrm = lambda k, shape, s: jax.random.normal(k, shape, f32) * s
    x = nrm(ks[0], (BATCH, SEQ, D), 1.0)
    c = nrm(ks[1], (BATCH, D), 1.0)
    offset = jax.random.randint(ks[2], (BATCH, 1), 0, 4096, dtype=jnp.int32)
    positions = jnp.arange(SEQ, dtype=jnp.int32)[None, :] + offset
    return {
        "x": x,
        "c": c,
        "positions": positions,
        "w_ada": nrm(ks[3], (D, 6 * D), D ** -0.5),
        "b_ada": nrm(ks[4], (6 * D,), 0.02),
        "g_mix": 1.0 + nrm(ks[5], (D,), 0.1),
        "w_in": nrm(ks[6], (D, IN_WIDTH), D ** -0.5),
        "b_in": nrm(ks[7], (IN_WIDTH,), 0.02),
        "attn_sinks": nrm(ks[8], (N_Q_HEADS,), 0.5),
        "conv_w": nrm(ks[9], (CONV_SIZE, CONV_WIDTH), CONV_SIZE ** -0.5),
        "conv_b": nrm(ks[10], (CONV_WIDTH,), 0.02),
        "conv_ln_g": 1.0 + nrm(ks[11], (CONV_WIDTH,), 0.1),
        "conv_ln_b": nrm(ks[12], (CONV_WIDTH,), 0.02),
        "w_out": nrm(ks[13], (D, D), D ** -0.5),
        "b_out": nrm(ks[14], (D,), 0.02),
        "g_ffn": 1.0 + nrm(ks[15], (D,), 0.1),
        "w_group_router": nrm(ks[16], (D, N_GROUPS), D ** -0.5),
        "b_group_router": nrm(ks[17], (N_GROUPS,), 0.01),
        "w_expert_router": nrm(ks[18], (D, N_EXPERTS), D ** -0.5),
        "b_expert_router": nrm(ks[19], (N_EXPERTS,), 0.01),
        "w_gate_up": nrm(ks[20], (N_EXPERTS, D, 2 * EXPERT_FF), D ** -0.5),
        "w_down": nrm(ks[21], (N_EXPERTS, EXPERT_FF, D), EXPERT_FF ** -0.5),
        "g_final": 1.0 + nrm(ks[22], (D,), 0.1),
    }


def reference(x, c, positions, w_ada, b_ada, g_mix, w_in, b_in, attn_sinks, conv_w, conv_b,
              conv_ln_g, conv_ln_b, w_out, b_out, g_ffn, w_group_router, b_group_router,
              w_expert_router, b_expert_router, w_gate_up, w_down, g_final):
    B, S, D = x.shape
    mod = jax.nn.silu(c) @ w_ada + b_ada
    shift1, scale1, gate1, shift2, scale2, gate2 = jnp.split(mod, 6, axis=-1)
    for _ in range(DEPTH):
        h = modulate(rmsnorm(x, g_mix), shift1, scale1)
        z = h @ w_in + b_in
        o = 0
        q = z[..., o:o + ATTN_WIDTH].reshape(B, S, N_Q_HEADS, HEAD_DIM); o += ATTN_WIDTH
        k = z[..., o:o + KV_WIDTH].reshape(B, S, N_KV_HEADS, HEAD_DIM); o += KV_WIDTH
        v = z[..., o:o + KV_WIDTH].reshape(B, S, N_KV_HEADS, HEAD_DIM); o += KV_WIDTH
        glu_a = z[..., o:o + CONV_WIDTH]; o += CONV_WIDTH
        glu_b = z[..., o:o + CONV_WIDTH]
        q = apply_rope(q, positions)
        k = apply_rope(k, positions)
        attn_out = sliding_window_sink_attention(q, k, v, attn_sinks)
        conv_out = conformer_conv(glu_a, glu_b, conv_w, conv_b, conv_ln_g, conv_ln_b)
        mixed = jnp.concatenate([attn_out, conv_out], axis=-1) @ w_out + b_out
        x = x + gate1[:, None, :] * mixed
        h2 = modulate(rmsnorm(x, g_ffn), shift2, scale2)
        ffn = hierarchical_moe(h2, w_group_router, b_group_router, w_expert_router,
                               b_expert_router, w_gate_up, w_down)
        x = x + gate2[:, None, :] * ffn
    return rmsnorm(x, g_final)
```

```python
import numpy as np
from contextlib import ExitStack
import concourse.bass as bass
import concourse.mybir as mybir
from concourse.bass_utils import run_bass_kernel_spmd

F32 = mybir.dt.float32
BF16 = mybir.dt.bfloat16
I32 = mybir.dt.int32
U32 = mybir.dt.uint32
AF = mybir.ActivationFunctionType
ALU = mybir.AluOpType
AX = mybir.AxisListType

NCORES = 8
D = 2048
KC = 16
NT = 2048
NH = 128
NTT = NT + NH
TT = NTT // 128
NQ = 16
DH = 64
CONVC = 1024
CSZ = 31
NE = 64
FF = 512
NBLK = 96
SEM_LIMIT = 30000


class Eng:
    def __init__(self, k, eng, name):
        self.k = k
        self.eng = eng
        self.name = name
        self.nsem = 0
        self.new_sem()
        self.waited = {}

    def new_sem(self):
        self.sem = self.k.es.enter_context(self.k.nc.semaphore(f"s_{self.name}_{self.nsem}"))
        self.nsem += 1
        self.cnt = 0

    def wait(self, tk):
        if tk is None:
            return
        sem, val = tk[0], tk[1]
        key = id(sem)
        if self.waited.get(key, 0) >= val:
            return
        self.eng.wait_ge(sem, val)
        self.waited[key] = val

    def done(self, ins):
        if self.cnt >= SEM_LIMIT:
            self.new_sem()
        self.cnt += 1
        ins.then_inc(self.sem, 1)
        return (self.sem, self.cnt, False)


class Buf:
    def __init__(self, name=""):
        self.name = name
        self.ws = []
        self.readers = []


class DmaQ:
    def __init__(self, k, e, nsem, name):
        self.k = k
        self.e = e
        self.sems = [k.es.enter_context(k.nc.semaphore(f"d_{name}_{i}")) for i in range(nsem)]
        self.cnt = [0] * nsem
        self.i = 0

    def next(self):
        i = self.i
        self.i = (self.i + 1) % len(self.sems)
        if self.cnt[i] > 0:
            self.e.wait((self.sems[i], 16 * self.cnt[i]))
        self.cnt[i] += 1
        return self.sems[i], 16 * self.cnt[i]


class K:
    def __init__(self, nc, es):
        self.nc = nc
        self.es = es
        self.pe = Eng(self, nc.tensor, "pe")
        self.act = Eng(self, nc.scalar, "act")
        self.dve = Eng(self, nc.vector, "dve")
        self.pool = Eng(self, nc.gpsimd, "pool")
        self.sp = Eng(self, nc.sync, "sp")
        self.engs = [self.pe, self.act, self.dve, self.pool, self.sp]
        self.q_sp = DmaQ(self, self.sp, 16, "sp")
        self.q_pool = DmaQ(self, self.pool, 16, "pool")
        self.q_act = DmaQ(self, self.act, 4, "act")

    def op(self, e, fn, R=(), W=()):
        for b in R:
            for t in b.ws:
                e.wait(t)
        for b in W:
            for t in b.ws:
                e.wait(t)
            for r in b.readers:
                e.wait(r)
        ins = fn()
        tk = e.done(ins)
        for b in R:
            b.readers.append(tk)
        for b in W:
            b.ws = [tk]
            b.readers = []
        return tk

    def dma(self, q, out, in_, R=(), W=(), indirect=None, **kw):
        e = q.e
        for b in R:
            for t in b.ws:
                e.wait(t)
        app = []
        for b in W:
            a = (len(b.ws) > 0 and not b.readers and all(t[2] for t in b.ws))
            app.append(a)
            if not a:
                for t in b.ws:
                    e.wait(t)
                for r in b.readers:
                    e.wait(r)
        sem, val = q.next()
        if indirect is None:
            ins = e.eng.dma_start(out=out, in_=in_, **kw)
        else:
            ins = e.eng.indirect_dma_start(out=out, in_=in_, **indirect)
        ins.then_inc(sem, 16)
        tk = (sem, val, True)
        for b in R:
            b.readers.append(tk)
        for b, a in zip(W, app):
            if a:
                b.ws.append(tk)
            else:
                b.ws = [tk]
                b.readers = []
        return tk

    def barrier(self):
        tks = [(e.sem, e.cnt) for e in self.engs if e.cnt > 0]
        for q in (self.q_sp, self.q_pool, self.q_act):
            for s, c in zip(q.sems, q.cnt):
                if c > 0:
                    tks.append((s, 16 * c))
        for e in self.engs:
            for tk in tks:
                if tk[0] is e.sem:
                    continue
                e.wait(tk)


SB_BASE = 17408
SB_LIMIT = 228352


class Arena:
    def __init__(self, nc):
        self.nc = nc
        self.ptr = SB_BASE
        self.top = SB_LIMIT
        self.n = 0

    def alloc(self, name, shape, dt=F32):
        sz = int(np.prod(shape[1:])) * (2 if dt == BF16 else 4)
        sz = (sz + 31) // 32 * 32
        off = self.ptr
        self.ptr += sz
        assert self.ptr <= self.top, f"SBUF overflow at {name}: {self.ptr} > {self.top}"
        self.n += 1
        return self.nc.alloc_sbuf_tensor_at(f"{name}_{self.n}", list(shape), dt, offset=off)

    def alloc_top(self, name, shape, dt=F32):
        sz = int(np.prod(shape[1:])) * (2 if dt == BF16 else 4)
        sz = (sz + 31) // 32 * 32
        self.top -= sz
        assert self.ptr <= self.top, f"SBUF overflow at {name}"
        self.n += 1
        return self.nc.alloc_sbuf_tensor_at(f"{name}_{self.n}", list(shape), dt, offset=self.top)


PI = float(np.pi)
import os as _os
CSTOP = int(_os.environ.get('CSTOP', '0'))


def build_program(debug=None):
    nc = bass.Bass("TRN2", target_bir_lowering=False)

    def din(name, shape, dt=F32):
        return nc.dram_tensor(name, list(shape), dt, kind="ExternalInput").ap()

    def dscratch(name, shape, dt=F32):
        return nc.dram_tensor(name, list(shape), dt, kind="Internal").ap()

    xh = din("xh", [NTT, D])
    cT = din("cT", [128, KC])
    w_ada = din("w_ada", [D, 6 * D])
    b_adaT = din("b_adaT", [128, 96])
    gvecT = din("gvecT", [128, 3 * KC])
    identf_d = din("identf", [128, 128])
    w_in_c = din("w_in_c", [37, 128, D])
    b_inT = din("b_inT", [128, 36])
    bv_d = din("bv", [128])
    pos_d = din("pos", [NTT], I32)
    smallc = din("smallc", [128, 4])
    masks_d = din("masks", [2, 128, 256])
    sinks_d = din("sinks", [NQ])
    cw_d = din("cw", [128, 8 * CSZ])
    cvec_d = din("cvec", [128, 24])
    w_out_d = din("w_out", [D, D])
    w_r_d = din("w_r", [128, KC * 72])
    b_r_d = din("b_r", [72])
    gfin_d = din("g_final", [D])
    iota_d = din("iotas", [128, 96 + 16 + 4])
    utri_d = din("utri", [128, 128])
    w_gu_d = din("w_gu", [NE * D, 2 * FF])
    w_dn_d = din("w_dn", [NE * FF, D])
    out = nc.dram_tensor("out", [NT, D], F32, kind="ExternalOutput").ap()
    h2_d = dscratch("h2_d", [NT, D], BF16)
    xin_d = dscratch("xin_d", [NBLK * 128, D], BF16)
    ys_d = dscratch("ys_d", [NBLK * 128, D])
    modvec = dscratch("modvec", [5, D])
    x1_d = dscratch("x1_d", [NT, D])
    if debug:
        dbg_q = nc.dram_tensor("dbg_q", [128, 8 * NT], BF16, kind="ExternalOutput").ap()
        dbg_k = nc.dram_tensor("dbg_k", [128, 2 * NTT], BF16, kind="ExternalOutput").ap()
        dbg_cat = nc.dram_tensor("dbg_cat", [128, 16 * NT], BF16, kind="ExternalOutput").ap()
        dbg_mod = nc.dram_tensor("dbg_mod", [128, 96], F32, kind="ExternalOutput").ap()
        dbg_rt = nc.dram_tensor("dbg_rt", [128, 16 * 132], F32, kind="ExternalOutput").ap()
        dbg_g = nc.dram_tensor("dbg_g", [128, 64 + 96 + 32], F32, kind="ExternalOutput").ap()

    with ExitStack() as es:
        k = K(nc, es)
        pe, act, dve, pool, sp = k.pe, k.act, k.dve, k.pool, k.sp
        A = Arena(nc)
        psall = nc.alloc_psum_tensor("psall", [128, 4096], F32)
        psb = [Buf(f"ps{i}") for i in range(8)]

        def bank(i, n=1):
            return psall[:, i * 512:(i + n) * 512]

        def bank_bf(i):
            return psall[:, i * 512:(i + 1) * 512].bitcast(BF16)

        ident_f = A.alloc("ident_f", [128, 128])
        ident_bf = A.alloc("ident_bf", [128, 128], BF16)
        ones_f = A.alloc("ones_f", [128, 128])
        modT = A.alloc("modT", [128, 96])
        gs1T = A.alloc("gs1T", [128, KC])
        gvT = A.alloc("gvT", [128, 3 * KC])
        smc = A.alloc("smc", [128, 4])
        sink_bc = A.alloc("sink_bc", [128, NQ])
        negsink = A.alloc("negsink", [128, NQ])
        bvbc = A.alloc("bvbc", [128, 128])
        binT = A.alloc("binT", [128, 36])
        cvec = A.alloc("cvec", [128, 24])
        cw = A.alloc("cw", [128, 8 * CSZ])
        mask_s = A.alloc("mask_s", [128, 256])
        mask_f = A.alloc("mask_f", [128, 256])
        mvf = A.alloc("mvf", [128, 80])
        mvs = A.alloc("mvs", [128, 128])
        b_const = Buf("const")
        for (dst, src) in ((ident_f[:], identf_d[:, :]), (gvT[:], gvecT[:, :]), (smc[:], smallc[:, :]),
                           (sink_bc[:], sinks_d.partition_broadcast(128)), (bvbc[:], bv_d.partition_broadcast(128)),
                           (binT[:], b_inT[:, :]), (cvec[:], cvec_d[:, :]), (cw[:], cw_d[:, :]),
                           (mask_s[:], masks_d[0]), (mask_f[:], masks_d[1])):
            k.dma(k.q_sp, dst, src, W=[b_const])
        k.op(dve, lambda: nc.vector.tensor_copy(out=ident_bf[:], in_=ident_f[:]), R=[b_const], W=[b_const])
        k.op(dve, lambda: nc.vector.memset(ones_f[:], 1.0), W=[b_const])
        k.op(dve, lambda: nc.vector.tensor_scalar(out=negsink[:], in0=sink_bc[:], scalar1=-1.0, scalar2=None, op0=ALU.mult),
             W=[b_const])
        invf = smc[:, 0:1]
        sgn = smc[:, 1:2]
        flag = smc[:, 2:3]
        P_END = A.ptr

        b_mod = Buf("mod")
        cT_s = A.alloc("cT_s", [128, KC])
        sT = A.alloc("sT", [128, KC], BF16)
        badaT = A.alloc("badaT", [128, 96])
        wada = [A.alloc(f"wada{i}", [128, 6 * D], BF16) for i in range(2)]
        b_w = [Buf("wada0"), Buf("wada1")]
        b_c = Buf("c")
        k.dma(k.q_sp, cT_s[:], cT[:, :], W=[b_c])
        k.dma(k.q_sp, badaT[:], b_adaT[:, :], W=[b_c])
        k.op(act, lambda: nc.scalar.activation(out=sT[:], in_=cT_s[:], func=AF.Silu), R=[b_c], W=[b_c])
        for kc in range(KC):
            w = wada[kc % 2]
            bw = b_w[kc % 2]
            for half in range(2):
                k.dma(k.q_pool, w[:, half * 6144:(half + 1) * 6144],
                      w_ada[kc * 128:(kc + 1) * 128, half * 6144:(half + 1) * 6144], W=[bw])

            def mm(kc=kc, w=w):
                ins = None
                for j in range(96):
                    ins = nc.tensor.matmul(psall[:, j:j + 1], lhsT=w[:, j * 128:(j + 1) * 128],
                                           rhs=sT[:, kc:kc + 1], start=(kc == 0 and j == 0),
                                           stop=(kc == KC - 1), skip_group_check=True)
                return ins
            k.op(pe, mm, R=[bw, b_c], W=[psb[0]])
        k.op(dve, lambda: nc.vector.tensor_tensor(out=modT[:], in0=psall[:, 0:96], in1=badaT[:], op=ALU.add),
             R=[psb[0], b_c], W=[b_mod])
        k.op(dve, lambda: nc.vector.scalar_tensor_tensor(out=gs1T[:], in0=modT[:, 16:32], scalar=1.0,
                                                         in1=gvT[:, 0:KC], op0=ALU.add, op1=ALU.mult),
             R=[b_mod, b_const], W=[b_mod])
        b_mv = Buf("mv")
        k.op(dve, lambda: nc.vector.tensor_copy(out=mvf[:, 0:16], in_=modT[:, 32:48]), R=[b_mod], W=[b_mv])
        k.op(dve, lambda: nc.vector.tensor_tensor(out=mvf[:, 16:32], in0=modT[:, 32:48], in1=gvT[:, 32:48], op=ALU.mult),
             R=[b_mod, b_const], W=[b_mv])
        k.op(dve, lambda: nc.vector.scalar_tensor_tensor(out=mvf[:, 32:48], in0=modT[:, 64:80], scalar=1.0,
                                                         in1=gvT[:, 16:32], op0=ALU.add, op1=ALU.mult),
             R=[b_mod, b_const], W=[b_mv])
        k.op(dve, lambda: nc.vector.tensor_copy(out=mvf[:, 48:64], in_=modT[:, 48:64]), R=[b_mod], W=[b_mv])
        k.op(dve, lambda: nc.vector.tensor_copy(out=mvf[:, 64:80], in_=modT[:, 80:96]), R=[b_mod], W=[b_mv])
        k.op(pe, lambda: nc.tensor.transpose(psall[0:80, 512:640], mvf[:, 0:80], ident_f[:]), R=[b_mv, b_const], W=[psb[1]])
        k.op(dve, lambda: nc.vector.tensor_copy(out=mvs[0:80, :], in_=psall[0:80, 512:640]), R=[psb[1]], W=[b_mv])
        k.dma(k.q_sp, modvec.rearrange("v (j p) -> (v j) p", p=128), mvs[0:80, :], R=[b_mv])
        if debug:
            k.dma(k.q_sp, dbg_mod[:, :], modT[:], R=[b_mod])
        k.barrier()
        A.ptr = P_END

        uT = A.alloc("uT", [128, 8, NTT], BF16)
        qT = A.alloc("qT", [128, 8, NT], BF16)
        Kd = [A.alloc(f"Kd{g}", [128, NTT], BF16) for g in range(2)]
        V = A.alloc("V", [128, TT, 128], BF16)
        b_uT, b_qT, b_Kd, b_V = Buf("uT"), Buf("qT"), Buf("Kd"), Buf("V")
        M1 = A.ptr

        cosT = A.alloc("cosT", [128, NTT])
        sinT = A.alloc("sinT", [128, NTT])
        b_tab = Buf("tab")
        M2 = A.ptr
        posi = A.alloc("posi", [128, NTT], I32)
        ang = A.alloc("ang", [128, NTT])
        tq = A.alloc("tq", [128, NTT])
        C1 = 6.28125
        C2 = 2 * PI - C1
        k.dma(k.q_sp, posi[:], pos_d.partition_broadcast(128), W=[b_tab])
        k.op(dve, lambda: nc.vector.tensor_copy(out=ang[:], in_=posi[:]), W=[b_tab])
        k.op(dve, lambda: nc.vector.tensor_scalar(out=ang[:], in0=ang[:], scalar1=invf, scalar2=None, op0=ALU.mult),
             R=[b_const], W=[b_tab])
        for (dst, off) in ((sinT, 0.0), (cosT, 0.5 * PI)):
            k.op(dve, lambda: nc.vector.tensor_scalar(out=dst[:], in0=ang[:], scalar1=off, scalar2=None, op0=ALU.add), W=[b_tab])
            k.op(dve, lambda: nc.vector.tensor_scalar(out=tq[:], in0=dst[:], scalar1=1.0 / (2 * PI), scalar2=0.5,
                                                      op0=ALU.mult, op1=ALU.add), W=[b_tab])
            k.op(dve, lambda: nc.vector.tensor_copy(out=posi[:], in_=tq[:]), W=[b_tab])
            k.op(dve, lambda: nc.vector.tensor_copy(out=tq[:], in_=posi[:]), W=[b_tab])
            k.op(dve, lambda: nc.vector.scalar_tensor_tensor(out=dst[:], in0=tq[:], scalar=-C1, in1=dst[:],
                                                             op0=ALU.mult, op1=ALU.add), W=[b_tab])
            k.op(dve, lambda: nc.vector.scalar_tensor_tensor(out=dst[:], in0=tq[:], scalar=-C2, in1=dst[:],
                                                             op0=ALU.mult, op1=ALU.add), W=[b_tab])
            k.op(dve, lambda: nc.vector.tensor_scalar(out=tq[:], in0=dst[:], scalar1=-PI, scalar2=None, op0=ALU.is_lt), W=[b_tab])
            k.op(dve, lambda: nc.vector.scalar_tensor_tensor(out=dst[:], in0=tq[:], scalar=2 * PI, in1=dst[:],
                                                             op0=ALU.mult, op1=ALU.add), W=[b_tab])
            k.op(dve, lambda: nc.vector.tensor_scalar(out=tq[:], in0=dst[:], scalar1=PI, scalar2=None, op0=ALU.is_gt), W=[b_tab])
            k.op(dve, lambda: nc.vector.scalar_tensor_tensor(out=dst[:], in0=tq[:], scalar=-2 * PI, in1=dst[:],
                                                             op0=ALU.mult, op1=ALU.add), W=[b_tab])
            k.op(dve, lambda: nc.vector.tensor_scalar(out=dst[:], in0=dst[:], scalar1=-3.14159, scalar2=3.14159,
                                                      op0=ALU.max, op1=ALU.min), W=[b_tab])
            k.op(act, lambda: nc.scalar.activation(out=dst[:], in_=dst[:], func=AF.Sin), W=[b_tab])
        k.op(dve, lambda: nc.vector.tensor_scalar(out=sinT[:], in0=sinT[:], scalar1=sgn, scalar2=None, op0=ALU.mult),
             R=[b_const], W=[b_tab])
        k.barrier()
        A.ptr = M2
        if debug == 'T':
            return nc

        HTOK = 1152
        hT = A.alloc("hT", [128, KC, HTOK], BF16)
        b_hT = Buf("hT")
        xt = [A.alloc(f"xt{i}", [128, D]) for i in range(2)]
        b_xt = [Buf(), Buf()]
        xs = [A.alloc(f"xs{i}", [128, D], BF16) for i in range(2)]
        b_xs = [Buf(), Buf()]
        junk = A.alloc("junkA", [128, D], BF16)
        b_junk = Buf()
        ssq = [A.alloc(f"ssq{i}", [128, 1]) for i in range(2)]
        b_ss = [Buf(), Buf()]
        wch = [A.alloc(f"wch{i}", [128, 2, D], BF16) for i in range(2)]
        b_wch = [Buf(), Buf()]
        tmp1 = [A.alloc(f"tmp1_{i}", [128, 512]) for i in range(2)]
        tmp2 = [A.alloc(f"tmp2_{i}", [128, 512]) for i in range(2)]
        b_tmp = [Buf(), Buf()]
        wv = A.alloc("wv", [128, D], BF16)
        b_wv = Buf()
        pair_ctr = [0]
        halves = [(0, 9, [(0, 128), (128, 640), (640, 1152)]), (9, 17, [(1152, 1664), (1664, 2176)])]
        pairs = [(c, 8 + c, 'q', c) for c in range(8)] + [(16, 17, 'k', 0), (18, 19, 'k', 1)] + \
                [(20 + c, 28 + c, 'glu', c) for c in range(8)]
        for (t_lo, t_hi, groups) in halves:
            tok0 = t_lo * 128
            for t in range(t_lo, t_hi):
                i = t % 2
                lt = (t - t_lo) * 128
                k.dma(k.q_sp, xt[i][:], xh[t * 128:(t + 1) * 128, :], W=[b_xt[i]])
                k.op(act, lambda: nc.scalar.activation(out=junk[:], in_=xt[i][:], func=AF.Square, accum_out=ssq[i][:]),
                     R=[b_xt[i]], W=[b_junk, b_ss[i]])
                k.op(act, lambda: nc.scalar.activation(out=ssq[i][:], in_=ssq[i][:], func=AF.Sqrt, scale=1.0 / D, bias=1e-6),
                     W=[b_ss[i]])
                k.op(dve, lambda: nc.vector.reciprocal(out=ssq[i][:], in_=ssq[i][:]), W=[b_ss[i]])
                k.op(act, lambda: nc.scalar.activation(out=xs[i][:], in_=xt[i][:], func=AF.Copy, scale=ssq[i][:, 0:1]),
                     R=[b_xt[i], b_ss[i]], W=[b_xs[i]])
                for hf in range(2):
                    pb = 6 + hf
                    pv = bank_bf(pb)

                    def tr(hf=hf, pv=pv, i=i):
                        ins = None
                        for j in range(8):
                            c = hf * 8 + j
                            ins = nc.tensor.transpose(pv[:, j * 128:(j + 1) * 128], xs[i][:, c * 128:(c + 1) * 128], ident_bf[:])
                        return ins
                    k.op(pe, tr, R=[b_xs[i], b_const], W=[psb[pb]])
                    for j in range(8):
                        c = hf * 8 + j
                        k.op(dve, lambda: nc.vector.tensor_scalar(out=hT[:, c, lt:lt + 128],
                                                                  in0=pv[:, j * 128:(j + 1) * 128],
                                                                  scalar1=gs1T[:, c:c + 1], scalar2=modT[:, c:c + 1],
                                                                  op0=ALU.mult, op1=ALU.add),
                             R=[psb[pb], b_mod], W=[b_hT])
            for (chA, chB, kind, c) in pairs:
                pi = pair_ctr[0] % 2
                pair_ctr[0] += 1
                w2 = wch[pi]
                k.dma(k.q_pool, w2[:, 0, :], w_in_c[chA], W=[b_wch[pi]])
                k.dma(k.q_pool, w2[:, 1, :], w_in_c[chB], W=[b_wch[pi]])
                for (g0, g1) in groups:
                    if kind == 'q' and g0 == 0:
                        continue
                    n = g1 - g0
                    l0 = g0 - tok0
                    pa, pb_ = (0, 1) if (pair_ctr[0] + g0 // 512) % 2 == 0 else (2, 3)
                    pair_ctr[0] += 0
                    for (which, pbank) in ((0, pa), (1, pb_)):
                        def mm(which=which, pbank=pbank, n=n, l0=l0, w2=w2):
                            ins = None
                            for kc in range(KC):
                                ins = nc.tensor.matmul(bank(pbank)[:, 0:n], lhsT=w2[:, which, kc * 128:(kc + 1) * 128],
                                                       rhs=hT[:, kc, l0:l0 + n], start=(kc == 0), stop=(kc == KC - 1))
                            return ins
                        k.op(pe, mm, R=[b_wch[pi], b_hT], W=[psb[pbank]])
                    ti = (g0 // 512) % 2
                    PA = bank(pa)[:, 0:n]
                    PB = bank(pb_)[:, 0:n]
                    if kind in ('q', 'k'):
                        k.op(dve, lambda: nc.vector.scalar_tensor_tensor(out=tmp1[ti][:, 0:n], in0=PA, scalar=binT[:, chA:chA + 1],
                                                                         in1=cosT[:, g0:g1], op0=ALU.add, op1=ALU.mult),
                             R=[psb[pa], b_const, b_tab], W=[b_tmp[ti]])
                        k.op(dve, lambda: nc.vector.scalar_tensor_tensor(out=tmp2[ti][:, 0:n], in0=PB, scalar=binT[:, chB:chB + 1],
                                                                         in1=sinT[:, g0:g1], op0=ALU.add, op1=ALU.mult),
                             R=[psb[pb_], b_const, b_tab], W=[b_tmp[ti]])
                        if kind == 'q':
                            dst = qT[:, c, g0 - NH:g1 - NH]
                            bdst = b_qT
                        else:
                            dst = Kd[c][:, g0:g1]
                            bdst = b_Kd
                        k.op(pool, lambda: nc.gpsimd.tensor_tensor(out=dst, in0=tmp1[ti][:, 0:n], in1=tmp2[ti][:, 0:n], op=ALU.add),
                             R=[b_tmp[ti]], W=[bdst])
                    else:
                        k.op(act, lambda: nc.scalar.activation(out=tmp2[ti][:, 0:n], in_=PB, func=AF.Sigmoid,
                                                               bias=binT[:, chB:chB + 1], scale=1.0),
                             R=[psb[pb_], b_const], W=[b_tmp[ti]])
                        if g0 == 0:
                            k.op(dve, lambda: nc.vector.scalar_tensor_tensor(out=tmp1[ti][:, 0:n], in0=PA, scalar=binT[:, chA:chA + 1],
                                                                             in1=tmp2[ti][:, 0:n], op0=ALU.add, op1=ALU.mult),
                                 R=[psb[pa], b_const], W=[b_tmp[ti]])
                            k.op(dve, lambda: nc.vector.tensor_scalar(out=uT[:, c, g0:g1], in0=tmp1[ti][:, 0:n], scalar1=flag,
                                                                      scalar2=None, op0=ALU.mult),
                                 R=[b_tmp[ti], b_const], W=[b_uT])
                        else:
                            k.op(dve, lambda: nc.vector.scalar_tensor_tensor(out=uT[:, c, g0:g1], in0=PA, scalar=binT[:, chA:chA + 1],
                                                                             in1=tmp2[ti][:, 0:n], op0=ALU.add, op1=ALU.mult),
                                 R=[psb[pa], b_const, b_tmp[ti]], W=[b_uT])
            k.dma(k.q_pool, wv[:], w_in_c[36], W=[b_wv])
            for t in range(t_lo, t_hi):
                lt = (t - t_lo) * 128
                pbank = 4 + (t % 2)

                def mmv(lt=lt, pbank=pbank):
                    ins = None
                    for kc in range(KC):
                        ins = nc.tensor.matmul(bank(pbank)[:, 0:128], lhsT=hT[:, kc, lt:lt + 128],
                                               rhs=wv[:, kc * 128:(kc + 1) * 128], start=(kc == 0), stop=(kc == KC - 1))
                    return ins
                k.op(pe, mmv, R=[b_wv, b_hT], W=[psb[pbank]])
                k.op(dve, lambda: nc.vector.tensor_tensor(out=V[:, t, :], in0=bank(pbank)[:, 0:128], in1=bvbc[:], op=ALU.add),
                     R=[psb[pbank], b_const], W=[b_V])
        if debug:
            k.dma(k.q_sp, dbg_q[:, :], qT[:].rearrange("p c t -> p (c t)"), R=[b_qT])
            k.dma(k.q_sp, dbg_k[:, 0:NTT], Kd[0][:], R=[b_Kd])
            k.dma(k.q_sp, dbg_k[:, NTT:2 * NTT], Kd[1][:], R=[b_Kd])
        k.barrier()
        A.ptr = M1
        if debug == 'B':
            return nc

        catA = A.alloc_top("catA", [128, 8, NT], BF16)
        catC = A.alloc_top("catC", [128, 8, NT], BF16)
        b_catA, b_catC = Buf("catA"), Buf("catC")
        NB4 = 4
        Sm = [A.alloc(f"Sm{i}", [128, 4, 256]) for i in range(NB4)]
        Pm = [A.alloc(f"Pm{i}", [128, 4, 256], BF16) for i in range(NB4)]
        PTs = [A.alloc(f"PTs{i}", [128, 1024], BF16) for i in range(NB4)]
        osb = [A.alloc(f"osb{i}", [128, NQ * DH], BF16) for i in range(2)]
        sm_small = [A.alloc(f"sms{i}", [128, 32]) for i in range(NB4)]
        b_Sm, b_Pm, b_PTs, b_sms = [[Buf() for _ in range(NB4)] for _ in range(4)]
        b_osb = [Buf(), Buf()]
        NIT = 64
        zt = A.alloc("zt", [128, D], BF16)
        b_zt = Buf()
        k.op(pool, lambda: nc.gpsimd.memset(zt[:], 0.0), W=[b_zt])
        zero_tks = []
        for n in range(NBLK):
            zero_tks.append(k.dma(k.q_sp, xin_d[n * 128:(n + 1) * 128, :], zt[:], R=[b_zt]))

        def smv(i):
            sm = sm_small[i]
            return (sm[:, 0:4], sm[:, 4:8], sm[:, 8:12], sm[:, 12:16], sm[:, 16:20], sm[:, 20:24], sm[:, 24:28])

        def stA(it):
            s, hg = it // 4, it % 4
            i = it % NB4
            g = hg // 2
            sb0 = 2 * (it % 2)
            mask = mask_f if s == 0 else mask_s
            Sps = bank(sb0, 2)

            def mmS():
                ins = None
                for hl in range(4):
                    h = hg * 4 + (0, 2, 1, 3)[hl]
                    c, half = h // 2, h % 2
                    ins = nc.tensor.matmul(psall[:, sb0 * 512 + hl * 256: sb0 * 512 + (hl + 1) * 256],
                                           lhsT=qT[half * 64:(half + 1) * 64, c, s * 128:(s + 1) * 128],
                                           rhs=Kd[g][half * 64:(half + 1) * 64, s * 128:s * 128 + 256],
                                           start=True, stop=True)
                return ins
            k.op(pe, mmS, R=[b_qT, b_Kd], W=[psb[sb0], psb[sb0 + 1]])
            rmax, negm, rsum, dlt, esink, den, rr = smv(i)
            k.op(dve, lambda: nc.vector.scalar_tensor_tensor(
                out=Sm[i][:], in0=Sps.rearrange("p (h k) -> p h k", h=4), scalar=0.125,
                in1=mask[:].unsqueeze(1).broadcast_to([128, 4, 256]), op0=ALU.mult, op1=ALU.add),
                R=[psb[sb0], psb[sb0 + 1], b_const], W=[b_Sm[i]])
            k.op(dve, lambda: nc.vector.tensor_reduce(out=rmax, in_=Sm[i][:], axis=AX.X, op=ALU.max),
                 R=[b_Sm[i]], W=[b_sms[i]])
            k.op(dve, lambda: nc.vector.scalar_tensor_tensor(out=negm, in0=rmax, scalar=-1.0,
                                                             in1=negsink[:, hg * 4:hg * 4 + 4], op0=ALU.mult, op1=ALU.min),
                 R=[b_const], W=[b_sms[i]])
            k.op(dve, lambda: nc.vector.tensor_tensor(out=dlt, in0=negm, in1=negsink[:, hg * 4:hg * 4 + 4], op=ALU.subtract),
                 R=[b_const], W=[b_sms[i]])

        def stB(it):
            i = it % NB4
            rmax, negm, rsum, dlt, esink, den, rr = smv(i)
            for hl in range(4):
                k.op(act, lambda: nc.scalar.activation(out=Pm[i][:, hl, :], in_=Sm[i][:, hl, :], func=AF.Exp,
                                                       bias=negm[:, hl:hl + 1], scale=1.0, accum_out=rsum[:, hl:hl + 1]),
                     R=[b_Sm[i]], W=[b_Pm[i], b_sms[i]])
            k.op(act, lambda: nc.scalar.activation(out=esink, in_=dlt, func=AF.Exp), W=[b_sms[i]])
            k.op(dve, lambda: nc.vector.tensor_tensor(out=den, in0=rsum, in1=esink, op=ALU.add), W=[b_sms[i]])
            k.op(dve, lambda: nc.vector.reciprocal(out=rr, in_=den), W=[b_sms[i]])

        def stC(it):
            i = it % NB4
            ptb = 4 + (it % 2)
            PTp = bank_bf(ptb)

            def trP():
                ins = None
                for hl in range(4):
                    for blk in range(2):
                        o = (hl * 2 + blk) * 128
                        ins = nc.tensor.transpose(PTp[:, o:o + 128], Pm[i][:, hl, blk * 128:(blk + 1) * 128], ident_bf[:])
                return ins
            k.op(pe, trP, R=[b_Pm[i], b_const], W=[psb[ptb]])
            k.op(act, lambda: nc.scalar.copy(out=PTs[i][:], in_=PTp), R=[psb[ptb]], W=[b_PTs[i]])

        def stD(it):
            s, hg = it // 4, it % 4
            i = it % NB4
            g = hg // 2
            oi = s % 2
            rmax, negm, rsum, dlt, esink, den, rr = smv(i)

            def mmO():
                ins = None
                for hl in range(4):
                    for blk in range(2):
                        o = (hl * 2 + blk) * 128
                        ins = nc.tensor.matmul(psall[:, 6 * 512 + hl * 64: 6 * 512 + (hl + 1) * 64],
                                               lhsT=PTs[i][:, o:o + 128], rhs=V[:, s + blk, g * 64:(g + 1) * 64],
                                               start=(blk == 0), stop=(blk == 1))
                return ins
            k.op(pe, mmO, R=[b_PTs[i], b_V], W=[psb[6]])
            k.op(dve, lambda: nc.vector.tensor_tensor(
                out=osb[oi][:, hg * 256:(hg + 1) * 256].rearrange("p (a b d) -> p b a d", a=2, b=2),
                in0=psall[:, 6 * 512: 6 * 512 + 256].rearrange("p (b a d) -> p b a d", a=2, b=2),
                in1=rr.rearrange("p (b a) -> p b a", a=2).unsqueeze(3).broadcast_to([128, 2, 2, 64]), op=ALU.mult),
                R=[psb[6], b_sms[i]], W=[b_osb[oi]])
            if hg == 3:
                oTp = bank_bf(7)

                def trO():
                    ins = None
                    for c in range(8):
                        ins = nc.tensor.transpose(oTp[:, c * 128:(c + 1) * 128], osb[oi][:, c * 128:(c + 1) * 128], ident_bf[:])
                    return ins
                k.op(pe, trO, R=[b_osb[oi], b_const], W=[psb[7]])
                k.op(act, lambda: nc.scalar.copy(out=catA[:, :, s * 128:(s + 1) * 128],
                                                 in_=oTp.rearrange("p (c t) -> p c t", c=8)),
                     R=[psb[7]], W=[b_catA])

        for step in range(NIT + 3):
            if step < NIT:
                stA(step)
            if 0 <= step - 1 < NIT:
                stB(step - 1)
            if 0 <= step - 2 < NIT:
                stC(step - 2)
            if 0 <= step - 3 < NIT:
                stD(step - 3)
        k.barrier()
        if debug == 'C':
            return nc

        A.ptr = M1 - (32768 + 2 * 4352 + 4352)
        diag = A.alloc("diag", [128, 8, CSZ, 128], BF16)
        b_diag = Buf()
        ybuf = A.alloc("ybuf", [128, 8, 512])
        b_y = Buf()
        ysq = [A.alloc(f"ysq{i}", [128, 512]) for i in range(2)]
        b_ysq = [Buf(), Buf()]
        mean = A.alloc("mean", [128, 512])
        rstd = A.alloc("rstd", [128, 512])
        msq = A.alloc("msq", [128, 512])
        b_st = Buf()
        t1 = [A.alloc(f"t1_{i}", [128, 512]) for i in range(2)]
        b_t1 = [Buf(), Buf()]
        n_d = 0
        for cc in range(8):
            for j in range(CSZ):
                if n_d % 2 == 0:
                    k.op(dve, lambda: nc.vector.tensor_scalar(out=diag[:, cc, j, :], in0=ident_f[:],
                                                              scalar1=cw[:, cc * CSZ + j: cc * CSZ + j + 1], scalar2=None, op0=ALU.mult),
                         R=[b_const], W=[b_diag])
                else:
                    k.op(pool, lambda: nc.gpsimd.tensor_scalar(out=diag[:, cc, j, :], in0=ident_f[:],
                                                               scalar1=cw[:, cc * CSZ + j: cc * CSZ + j + 1], scalar2=1.0,
                                                               op0=ALU.mult, op1=ALU.mult),
                         R=[b_const], W=[b_diag])
                n_d += 1
        for g in range(4):
            s0 = g * 512
            for cc in range(8):
                pbank = cc % 4

                def mmC(cc=cc, pbank=pbank, s0=s0):
                    ins = None
                    for j in range(CSZ):
                        o = NH + s0 - (CSZ - 1) + j
                        ins = nc.tensor.matmul(bank(pbank), lhsT=diag[:, cc, j, :], rhs=uT[:, cc, o:o + 512],
                                               start=(j == 0), stop=(j == CSZ - 1))
                    return ins
                k.op(pe, mmC, R=[b_diag, b_uT], W=[psb[pbank]])
                k.op(act, lambda: nc.scalar.activation(out=ybuf[:, cc, :], in_=bank(pbank), func=AF.Identity,
                                                       bias=cvec[:, cc:cc + 1], scale=1.0),
                     R=[psb[pbank], b_const], W=[b_y])
            def mmS1():
                ins = None
                for cc in range(8):
                    ins = nc.tensor.matmul(bank(4), lhsT=ones_f[:], rhs=ybuf[:, cc, :], start=(cc == 0), stop=(cc == 7))
                return ins
            k.op(pe, mmS1, R=[b_y, b_const], W=[psb[4]])
            for cc in range(8):
                i = cc % 2
                k.op(act, lambda: nc.scalar.activation(out=ysq[i][:], in_=ybuf[:, cc, :], func=AF.Square),
                     R=[b_y], W=[b_ysq[i]])
                k.op(pe, lambda: nc.tensor.matmul(bank(5), lhsT=ones_f[:], rhs=ysq[i][:], start=(cc == 0), stop=(cc == 7),
                                                  skip_group_check=True),
                     R=[b_ysq[i], b_const], W=[psb[5]])
            k.op(dve, lambda: nc.vector.tensor_scalar(out=mean[:], in0=bank(4), scalar1=1.0 / CONVC, scalar2=None, op0=ALU.mult),
                 R=[psb[4]], W=[b_st])
            k.op(dve, lambda: nc.vector.tensor_tensor(out=msq[:], in0=mean[:], in1=mean[:], op=ALU.mult), W=[b_st])
            k.op(dve, lambda: nc.vector.scalar_tensor_tensor(out=rstd[:], in0=bank(5), scalar=1.0 / CONVC, in1=msq[:],
                                                             op0=ALU.mult, op1=ALU.subtract),
                 R=[psb[5]], W=[b_st])
            k.op(act, lambda: nc.scalar.activation(out=rstd[:], in_=rstd[:], func=AF.Sqrt, bias=1e-5, scale=1.0), W=[b_st])
            k.op(dve, lambda: nc.vector.reciprocal(out=rstd[:], in_=rstd[:]), W=[b_st])
            for cc in range(8):
                i = cc % 2
                k.op(dve, lambda: nc.vector.tensor_tensor(out=t1[i][:], in0=ybuf[:, cc, :], in1=mean[:], op=ALU.subtract),
                     R=[b_y, b_st], W=[b_t1[i]])
                k.op(pool, lambda: nc.gpsimd.tensor_tensor(out=t1[i][:], in0=t1[i][:], in1=rstd[:], op=ALU.mult),
                     R=[b_st], W=[b_t1[i]])
                k.op(act, lambda: nc.scalar.activation(out=catC[:, cc, s0:s0 + 512], in_=t1[i][:], func=AF.Silu,
                                                       scale=cvec[:, 8 + cc:9 + cc], bias=cvec[:, 16 + cc:17 + cc]),
                     R=[b_t1[i], b_const], W=[b_catC])
        if debug:
            k.dma(k.q_sp, dbg_cat[:, 0:8 * NT], catA[:].rearrange("p c t -> p (c t)"), R=[b_catA])
            k.dma(k.q_sp, dbg_cat[:, 8 * NT:16 * NT], catC[:].rearrange("p c t -> p (c t)"), R=[b_catC])
        k.barrier()
        if debug == 'D':
            return nc

        A.ptr = P_END
        w_o = A.alloc("w_o", [128, KC, D], BF16)
        b_wo = Buf()
        g1bc = A.alloc("g1bc", [128, D])
        gbbc = A.alloc("gbbc", [128, D])
        b_bc = Buf()
        xt2 = [A.alloc(f"xt2_{i}", [128, D]) for i in range(2)]
        x1t = [A.alloc(f"x1t_{i}", [128, D]) for i in range(2)]
        b_xt2, b_x1t = [Buf(), Buf()], [Buf(), Buf()]
        for kc in range(KC):
            k.dma(k.q_pool, w_o[:, kc, :], w_out_d[kc * 128:(kc + 1) * 128, :], W=[b_wo])
        k.dma(k.q_sp, g1bc[:], modvec[0].partition_broadcast(128), W=[b_bc])
        k.dma(k.q_sp, gbbc[:], modvec[1].partition_broadcast(128), W=[b_bc])
        k.dma(k.q_sp, xt2[0][:], xh[NH: NH + 128, :], W=[b_xt2[0]])
        for s in range(16):
            i = s % 2
            if s + 1 < 16:
                k.dma(k.q_sp, xt2[1 - i][:], xh[NH + (s + 1) * 128: NH + (s + 2) * 128, :], W=[b_xt2[1 - i]])
            k.op(pool, lambda: nc.gpsimd.tensor_tensor(out=xt2[i][:], in0=xt2[i][:], in1=gbbc[:], op=ALU.add),
                 R=[b_bc], W=[b_xt2[i]])
            for n in range(4):
                pbank = 4 * i + n

                def mmE(n=n, pbank=pbank, s=s):
                    ins = None
                    for kc in range(KC):
                        src = catA if kc < 8 else catC
                        ins = nc.tensor.matmul(bank(pbank), lhsT=src[:, kc % 8, s * 128:(s + 1) * 128],
                                               rhs=w_o[:, kc, n * 512:(n + 1) * 512], start=(kc == 0), stop=(kc == KC - 1))
                    return ins
                k.op(pe, mmE, R=[b_catA, b_catC, b_wo], W=[psb[pbank]])
                k.op(dve, lambda: nc.vector.tensor_tensor(out=x1t[i][:, n * 512:(n + 1) * 512], in0=bank(pbank),
                                                          in1=g1bc[:, n * 512:(n + 1) * 512], op=ALU.mult),
                     R=[psb[pbank], b_bc], W=[b_x1t[i]])
            k.op(pool, lambda: nc.gpsimd.tensor_tensor(out=x1t[i][:], in0=x1t[i][:], in1=xt2[i][:], op=ALU.add),
                 R=[b_xt2[i]], W=[b_x1t[i]])
            k.dma(k.q_sp, x1_d[s * 128:(s + 1) * 128, :], x1t[i][:], R=[b_x1t[i]])
            if debug == 'E':
                k.dma(k.q_sp, out[s * 128:(s + 1) * 128, :], x1t[i][:], R=[b_x1t[i]])
        k.barrier()
        if debug == 'E':
            return nc

        A.ptr = P_END
        A.top = SB_LIMIT
        ones_bf = A.alloc("ones_bf", [128, 128], BF16)
        utri = A.alloc("utri", [128, 128], BF16)
        utri_f = A.alloc("utri_f", [128, 128])
        iot = A.alloc("iot", [128, 96 + 16 + 4])
        w12all = A.alloc("w12all", [128, 16, 2])
        destf = A.alloc("destf", [128, 16, 2])
        desti = A.alloc("desti", [128, 16, 2], I32)
        idx_gu = A.alloc("idx_gu", [128, NBLK, 16], I32)
        idx_dn = A.alloc("idx_dn", [128, NBLK, 4], I32)
        b_rt = Buf("routing")
        b_c2 = Buf("const2")
        k.dma(k.q_sp, utri_f[:], utri_d[:, :], W=[b_c2])
        k.dma(k.q_sp, iot[:], iota_d[:, :], W=[b_c2])
        k.op(dve, lambda: nc.vector.tensor_copy(out=utri[:], in_=utri_f[:]), W=[b_c2])
        k.op(dve, lambda: nc.vector.memset(ones_bf[:], 1.0), W=[b_c2])
        H_KEEP = A.ptr
        O1all = A.alloc("O1all", [128, 16, 64])
        O2all = A.alloc("O2all", [128, 16, 64])
        O12all = A.alloc("O12all", [128, 16, 64], BF16)
        G_KEEP = A.ptr
        gs2bc = A.alloc("gs2bc", [128, D])
        sh2bc = A.alloc("sh2bc", [128, D])
        w_r = A.alloc("w_r", [128, KC, 72])
        brbc = A.alloc("brbc", [128, 72])
        k.dma(k.q_sp, gs2bc[:], modvec[2].partition_broadcast(128), W=[b_c2])
        k.dma(k.q_sp, sh2bc[:], modvec[3].partition_broadcast(128), W=[b_c2])
        k.dma(k.q_sp, w_r[:].rearrange("p c n -> p (c n)"), w_r_d[:, :], W=[b_c2])
        k.dma(k.q_sp, brbc[:], b_r_d.partition_broadcast(128), W=[b_c2])
        x1b = [A.alloc(f"x1b{i}", [128, D]) for i in range(2)]
        h2f = [A.alloc(f"h2f{i}", [128, D]) for i in range(2)]
        h2b = [A.alloc(f"h2b{i}", [128, D], BF16) for i in range(2)]
        h2T = A.alloc("h2T", [128, KC, 128])
        lg_all = A.alloc("lg_all", [128, 16, 72])
        rs = [A.alloc(f"rs{i}", [128, 8]) for i in range(2)]
        junk2 = A.alloc("junk2", [128, D], BF16)
        b_x1b, b_h2f, b_h2b, b_rs = [Buf(), Buf()], [Buf(), Buf()], [Buf(), Buf()], [Buf(), Buf()]
        b_h2T, b_junk2, b_lg = Buf(), Buf(), Buf()
        def e2_stage1(s):
            i = s % 2
            ss2 = rs[i][:, 0:1]
            k.dma(k.q_sp, x1b[i][:], x1_d[s * 128:(s + 1) * 128, :], W=[b_x1b[i]])
            k.op(act, lambda: nc.scalar.activation(out=junk2[:], in_=x1b[i][:], func=AF.Square, accum_out=ss2),
                 R=[b_x1b[i]], W=[b_junk2, b_rs[i]])
            k.op(act, lambda: nc.scalar.activation(out=ss2, in_=ss2, func=AF.Sqrt, scale=1.0 / D, bias=1e-6), W=[b_rs[i]])
            k.op(dve, lambda: nc.vector.reciprocal(out=ss2, in_=ss2), W=[b_rs[i]])
            k.op(dve, lambda: nc.vector.scalar_tensor_tensor(out=h2f[i][:], in0=x1b[i][:], scalar=ss2, in1=gs2bc[:],
                                                             op0=ALU.mult, op1=ALU.mult),
                 R=[b_x1b[i], b_rs[i], b_c2], W=[b_h2f[i]])
            k.op(pool, lambda: nc.gpsimd.tensor_tensor(out=h2f[i][:], in0=h2f[i][:], in1=sh2bc[:], op=ALU.add),
                 R=[b_c2], W=[b_h2f[i]])
            k.op(act, lambda: nc.scalar.copy(out=h2b[i][:], in_=h2f[i][:]), R=[b_h2f[i]], W=[b_h2b[i]])
            k.dma(k.q_sp, h2_d[s * 128:(s + 1) * 128, :], h2b[i][:], R=[b_h2b[i]])

        def e2_stage2(s):
            i = s % 2
            hT_ = h2T2[i]
            for q4 in range(4):
                def trH(q4=q4, i=i):
                    ins = None
                    for j in range(4):
                        c = q4 * 4 + j
                        ins = nc.tensor.transpose(psall[:, q4 * 512 + j * 128: q4 * 512 + (j + 1) * 128],
                                                  h2f[i][:, c * 128:(c + 1) * 128], ident_f[:])
                    return ins
                k.op(pe, trH, R=[b_h2f[i], b_const], W=[psb[q4]])
                if q4 % 2 == 0:
                    k.op(act, lambda: nc.scalar.copy(out=hT_[:, q4 * 4:(q4 + 1) * 4, :].rearrange("p c t -> p (c t)"), in_=bank(q4)),
                         R=[psb[q4]], W=[b_h2T2[i]])
                else:
                    k.op(dve, lambda: nc.vector.tensor_copy(out=hT_[:, q4 * 4:(q4 + 1) * 4, :].rearrange("p c t -> p (c t)"), in_=bank(q4)),
                         R=[psb[q4]], W=[b_h2T2[i]])
            pbl = 4 + (s % 2)

            def mmR(pbl=pbl):
                ins = None
                for kc in range(KC):
                    ins = nc.tensor.matmul(psall[:, pbl * 512: pbl * 512 + 72], lhsT=hT_[:, kc, :], rhs=w_r[:, kc, :],
                                           start=(kc == 0), stop=(kc == KC - 1))
                return ins
            k.op(pe, mmR, R=[b_h2T2[i], b_c2], W=[psb[pbl]])
            k.op(dve, lambda: nc.vector.tensor_tensor(out=lg_all[:, s, :], in0=psall[:, pbl * 512: pbl * 512 + 72], in1=brbc[:], op=ALU.add),
                 R=[psb[pbl], b_c2], W=[b_lg])

        h2T2 = [h2T, A.alloc("h2Tb", [128, KC, 128])]
        b_h2T2 = [Buf(), Buf()]
        e2_stage1(0)
        for s in range(16):
            if s + 1 < 16:
                e2_stage1(s + 1)
            e2_stage2(s)
        T16 = 16
        gmax = A.alloc("gmax", [128, T16]); ohg = A.alloc("ohg", [128, T16, 8]); ege = A.alloc("ege", [128, T16, 8])
        gsum = A.alloc("gsum", [128, T16]); pg = A.alloc("pg", [128, T16]); selt = A.alloc("selt", [128, T16, 8, 8])
        eg = A.alloc("eg", [128, T16, 8]); v1 = A.alloc("v1", [128, T16]); oh1 = A.alloc("oh1", [128, T16, 8])
        eg2 = A.alloc("eg2", [128, T16, 8]); v2 = A.alloc("v2", [128, T16]); oh2 = A.alloc("oh2", [128, T16, 8])
        d21 = A.alloc("d21", [128, T16]); wA = A.alloc("wA", [128, T16])
        gl = lg_all[:, :, 0:8]
        el4 = lg_all[:, :, 8:72].rearrange("p t (g j) -> p t g j", g=8)
        Wr = [b_rt]

        def bc3(ap2):
            return ap2.unsqueeze(2).broadcast_to([128, T16, 8])
        k.op(dve, lambda: nc.vector.tensor_reduce(out=gmax[:], in_=gl, axis=AX.X, op=ALU.max), R=[b_lg], W=Wr)
        k.op(dve, lambda: nc.vector.tensor_tensor(out=ohg[:], in0=gl, in1=bc3(gmax[:]), op=ALU.is_equal), R=[b_lg], W=Wr)
        k.op(dve, lambda: nc.vector.tensor_tensor(out=ege[:], in0=gl, in1=bc3(gmax[:]), op=ALU.subtract), R=[b_lg], W=Wr)
        k.op(act, lambda: nc.scalar.activation(out=ege[:], in_=ege[:], func=AF.Exp), W=Wr)
        k.op(dve, lambda: nc.vector.tensor_reduce(out=gsum[:], in_=ege[:], axis=AX.X, op=ALU.add), W=Wr)
        k.op(dve, lambda: nc.vector.reciprocal(out=pg[:], in_=gsum[:]), W=Wr)
        k.op(dve, lambda: nc.vector.tensor_tensor(out=selt[:], in0=el4, in1=ohg[:].unsqueeze(3).broadcast_to([128, T16, 8, 8]),
                                                  op=ALU.mult), R=[b_lg], W=Wr)
        k.op(dve, lambda: nc.vector.tensor_reduce(out=eg[:], in_=selt[:].rearrange("p t g j -> p t j g"), axis=AX.X, op=ALU.add), W=Wr)
        k.op(dve, lambda: nc.vector.tensor_reduce(out=v1[:], in_=eg[:], axis=AX.X, op=ALU.max), W=Wr)
        k.op(dve, lambda: nc.vector.tensor_tensor(out=oh1[:], in0=eg[:], in1=bc3(v1[:]), op=ALU.is_equal), W=Wr)
        k.op(dve, lambda: nc.vector.scalar_tensor_tensor(out=eg2[:], in0=oh1[:], scalar=-1e30, in1=eg[:], op0=ALU.mult, op1=ALU.add), W=Wr)
        k.op(dve, lambda: nc.vector.tensor_reduce(out=v2[:], in_=eg2[:], axis=AX.X, op=ALU.max), W=Wr)
        k.op(dve, lambda: nc.vector.tensor_tensor(out=oh2[:], in0=eg2[:], in1=bc3(v2[:]), op=ALU.is_equal), W=Wr)
        k.op(dve, lambda: nc.vector.tensor_tensor(out=d21[:], in0=v2[:], in1=v1[:], op=ALU.subtract), W=Wr)
        k.op(act, lambda: nc.scalar.activation(out=d21[:], in_=d21[:], func=AF.Exp), W=Wr)
        k.op(dve, lambda: nc.vector.tensor_scalar(out=d21[:], in0=d21[:], scalar1=1.0, scalar2=None, op0=ALU.add), W=Wr)
        k.op(dve, lambda: nc.vector.reciprocal(out=wA[:], in_=d21[:]), W=Wr)
        k.op(dve, lambda: nc.vector.tensor_tensor(out=w12all[:, :, 0], in0=pg[:], in1=wA[:], op=ALU.mult), W=Wr)
        k.op(dve, lambda: nc.vector.tensor_tensor(out=w12all[:, :, 1], in0=pg[:], in1=w12all[:, :, 0], op=ALU.subtract), W=Wr)
        O1v = O1all[:].rearrange("p t (g j) -> p t g j", g=8)
        O2v = O2all[:].rearrange("p t (g j) -> p t g j", g=8)
        k.op(dve, lambda: nc.vector.tensor_tensor(out=O1v, in0=ohg[:].unsqueeze(3).broadcast_to([128, T16, 8, 8]),
                                                  in1=oh1[:].unsqueeze(2).broadcast_to([128, T16, 8, 8]), op=ALU.mult), W=Wr)
        k.op(dve, lambda: nc.vector.tensor_tensor(out=O2v, in0=ohg[:].unsqueeze(3).broadcast_to([128, T16, 8, 8]),
                                                  in1=oh2[:].unsqueeze(2).broadcast_to([128, T16, 8, 8]), op=ALU.mult), W=Wr)
        k.op(dve, lambda: nc.vector.tensor_tensor(out=O12all[:], in0=O1all[:], in1=O2all[:], op=ALU.add), W=Wr)
        k.barrier()

        A.ptr = G_KEEP
        cnt = A.alloc("cnt", [128, 64])
        nbk = A.alloc("nbk", [128, 64])
        cs = [A.alloc(f"cs{i}", [128, 64]) for i in range(2)]
        pst = A.alloc("pst", [128, 64])
        bef = A.alloc("bef", [128, NBLK])
        cmp3 = A.alloc("cmp3", [128, NBLK, 64])
        tf1 = A.alloc("tf1", [128, NBLK, 16])
        b_g = Buf("g")

        def mmCnt():
            ins = None
            for s in range(16):
                ins = nc.tensor.matmul(psall[:, 0:64], lhsT=ones_bf[:], rhs=O12all[:, s, :], start=(s == 0), stop=(s == 15))
            return ins
        k.op(pe, mmCnt, R=[b_rt, b_c2], W=[psb[0]])
        k.op(dve, lambda: nc.vector.tensor_copy(out=cnt[:], in_=psall[:, 0:64]), R=[psb[0]], W=[b_g])
        k.op(dve, lambda: nc.vector.memset(nbk[:], 0.0), W=[b_g])
        for m in range(16):
            k.op(dve, lambda: nc.vector.scalar_tensor_tensor(out=nbk[:], in0=cnt[:], scalar=128.0 * m + 0.5, in1=nbk[:],
                                                             op0=ALU.is_gt, op1=ALU.add), W=[b_g])
        k.op(dve, lambda: nc.vector.tensor_copy(out=cs[0][:], in_=nbk[:]), W=[b_g])
        cur = 0
        for dsh in (1, 2, 4, 8, 16, 32):
            a_, b_ = cs[cur], cs[1 - cur]
            k.op(dve, lambda: nc.vector.tensor_copy(out=b_[:], in_=a_[:]), W=[b_g])
            k.op(dve, lambda: nc.vector.tensor_tensor(out=b_[:, dsh:64], in0=a_[:, dsh:64], in1=a_[:, 0:64 - dsh], op=ALU.add), W=[b_g])
            cur = 1 - cur
        bend = cs[cur]
        k.op(dve, lambda: nc.vector.tensor_tensor(out=pst[:], in0=bend[:], in1=nbk[:], op=ALU.subtract), W=[b_g])
        k.op(dve, lambda: nc.vector.tensor_scalar(out=pst[:], in0=pst[:], scalar1=128.0, scalar2=None, op0=ALU.mult), W=[b_g])
        k.op(dve, lambda: nc.vector.tensor_tensor(out=cmp3[:], in0=bend[:].unsqueeze(1).broadcast_to([128, NBLK, 64]),
                                                  in1=iot[:, 0:NBLK].unsqueeze(2).broadcast_to([128, NBLK, 64]), op=ALU.is_le),
             R=[b_c2], W=[b_g])
        k.op(dve, lambda: nc.vector.tensor_reduce(out=bef[:], in_=cmp3[:], axis=AX.X, op=ALU.add), W=[b_g])
        k.op(dve, lambda: nc.vector.scalar_tensor_tensor(out=tf1[:], in0=bef[:].unsqueeze(2).broadcast_to([128, NBLK, 16]), scalar=float(D),
                                                         in1=iot[:, 96:112].unsqueeze(1).broadcast_to([128, NBLK, 16]),
                                                         op0=ALU.mult, op1=ALU.add), R=[b_c2], W=[b_g])
        k.op(dve, lambda: nc.vector.tensor_copy(out=idx_gu[:], in_=tf1[:]), W=[b_g])
        k.op(dve, lambda: nc.vector.scalar_tensor_tensor(out=tf1[:, :, 0:4], in0=bef[:].unsqueeze(2).broadcast_to([128, NBLK, 4]), scalar=512.0,
                                                         in1=iot[:, 96:100].unsqueeze(1).broadcast_to([128, NBLK, 4]),
                                                         op0=ALU.mult, op1=ALU.add), R=[b_c2], W=[b_g])
        k.op(dve, lambda: nc.vector.tensor_copy(out=idx_dn[:], in_=tf1[:, :, 0:4]), W=[b_g])
        for s in range(16):
            pbank = 1 + s // 8
            col = pbank * 512 + (s % 8) * 64

            def mmRank(s=s, col=col):
                ins = nc.tensor.matmul(psall[:, col: col + 64], lhsT=utri[:], rhs=O12all[:, s, :],
                                       start=True, stop=(s == 0))
                for s2 in range(s):
                    ins = nc.tensor.matmul(psall[:, col: col + 64], lhsT=ones_bf[:], rhs=O12all[:, s2, :],
                                           start=False, stop=(s2 == s - 1))
                return ins
            k.op(pe, mmRank, R=[b_rt, b_c2], W=[psb[pbank]])
        prA = A.alloc("prA", [128, 16, 64])
        prB = A.alloc("prB", [128, 16, 64])
        k.op(dve, lambda: nc.vector.tensor_tensor(out=prA[:], in0=psall[:, 512:1536].rearrange("p (t e) -> p t e", e=64),
                                                  in1=pst[:].unsqueeze(1).broadcast_to([128, 16, 64]), op=ALU.add),
             R=[psb[1], psb[2]], W=[b_g])
        k.op(dve, lambda: nc.vector.tensor_tensor(out=prB[:], in0=prA[:], in1=O1all[:], op=ALU.mult), R=[b_rt], W=[b_g])
        k.op(dve, lambda: nc.vector.tensor_reduce(out=destf[:, :, 0], in_=prB[:], axis=AX.X, op=ALU.add), W=[b_g])
        k.op(dve, lambda: nc.vector.tensor_tensor(out=prB[:], in0=prA[:], in1=O2all[:], op=ALU.mult), R=[b_rt], W=[b_g])
        k.op(dve, lambda: nc.vector.tensor_reduce(out=destf[:, :, 1], in_=prB[:], axis=AX.X, op=ALU.add), W=[b_g])
        k.op(dve, lambda: nc.vector.tensor_copy(out=desti[:], in_=destf[:]), W=[b_g])
        if debug:
            for s in range(16):
                k.dma(k.q_sp, dbg_rt[:, s * 132: s * 132 + 64], O1all[:, s, :], R=[b_rt])
                k.dma(k.q_sp, dbg_rt[:, s * 132 + 64: s * 132 + 128], O2all[:, s, :], R=[b_rt])
                k.dma(k.q_sp, dbg_rt[:, s * 132 + 128: s * 132 + 130], w12all[:, s, :], R=[b_rt])
                k.dma(k.q_sp, dbg_rt[:, s * 132 + 130: s * 132 + 132], destf[:, s, :], R=[b_g])
            k.dma(k.q_sp, dbg_g[:, 0:64], cnt[:], R=[b_g])
            k.dma(k.q_sp, dbg_g[:, 64:160], bef[:], R=[b_g])
        k.barrier()
        if debug == 'G':
            return nc

        IOA = bass.IndirectOffsetOnAxis
        reg_gu = nc.gpsimd.alloc_register("bc_gu")
        nc.gpsimd.reg_mov(reg_gu, NE * D - 1)
        reg_dn = nc.gpsimd.alloc_register("bc_dn")
        nc.gpsimd.reg_mov(reg_dn, NE * FF - 1)
        A.ptr = H_KEEP
        NWB = 3
        wgu = [A.alloc(f"wgu{i}", [128, KC, 2 * FF], BF16) for i in range(NWB)]
        wdn = [A.alloc(f"wdn{i}", [128, 4, D], BF16) for i in range(NWB)]
        xin = [A.alloc(f"xin{i}", [128, D], BF16) for i in range(2)]
        xinT = [A.alloc(f"xinT{i}", [128, KC, 128], BF16) for i in range(2)]
        sgt = [A.alloc(f"sgt{i}", [128, 512]) for i in range(2)]
        hmT = [A.alloc(f"hmT{i}", [128, 4, 128], BF16) for i in range(2)]
        yst = [A.alloc(f"yst{i}", [128, D]) for i in range(2)]
        b_wgu, b_wdn, b_xin, b_xinT, b_sgt, b_hmT, b_yst = [[Buf(), Buf(), Buf()] for _ in range(7)]
        for t in zero_tks:
            pool.wait(t)
        sc_tks = []
        for s in range(16):
            i = s % 2
            k.dma(k.q_sp, xin[i][:], h2_d[s * 128:(s + 1) * 128, :], W=[b_xin[i]])
            for j in range(2):
                sc_tks.append(k.dma(k.q_pool, xin_d[:, :], xin[i][:], R=[b_xin[i], b_g],
                                    indirect=dict(out_offset=IOA(ap=desti[:, s, j:j + 1].bitcast(U32), axis=0), in_offset=None)))
        for t in sc_tks:
            sp.wait(t)
        ys_tks = []
        border = []
        for j in range(NBLK // 3):
            border += [2 * j, 2 * j + 1, NBLK - 1 - j]
        def h_loads(pos):
            b = border[pos]
            i = pos % 2
            wi = pos % NWB
            k.dma(k.q_sp, xin[i][:], xin_d[b * 128:(b + 1) * 128, :], W=[b_xin[i]])
            for j in range(KC):
                k.dma(k.q_pool, wgu[wi][:, j, :], w_gu_d[:, :], R=[b_g], W=[b_wgu[wi]],
                      indirect=dict(out_offset=None, in_offset=IOA(ap=idx_gu[:, b, j:j + 1].bitcast(U32), axis=0),
                                    bounds_check=reg_gu, oob_is_err=False))
            for kc in range(4):
                k.dma(k.q_pool, wdn[wi][:, kc, :], w_dn_d[:, :], R=[b_g], W=[b_wdn[wi]],
                      indirect=dict(out_offset=None, in_offset=IOA(ap=idx_dn[:, b, kc:kc + 1].bitcast(U32), axis=0),
                                    bounds_check=reg_dn, oob_is_err=False))

        def h_front(pos):
            i = pos % 2
            for hf in range(2):
                pv = bank_bf(hf)

                def trX(hf=hf, pv=pv, i=i):
                    ins = None
                    for j in range(8):
                        c = hf * 8 + j
                        ins = nc.tensor.transpose(pv[:, j * 128:(j + 1) * 128], xin[i][:, c * 128:(c + 1) * 128], ident_bf[:])
                    return ins
                k.op(pe, trX, R=[b_xin[i], b_const], W=[psb[hf]])
                dstv = xinT[i][:, hf * 8:(hf + 1) * 8, :].rearrange("p c t -> p (c t)")
                if hf == 0:
                    k.op(act, lambda: nc.scalar.copy(out=dstv, in_=pv), R=[psb[hf]], W=[b_xinT[i]])
                else:
                    k.op(dve, lambda: nc.vector.tensor_copy(out=dstv, in_=pv), R=[psb[hf]], W=[b_xinT[i]])

        def h_mid(pos):
            i = pos % 2
            wi = pos % NWB
            for gu in range(2):
                def mmGU(gu=gu, i=i, wi=wi):
                    ins = None
                    for m4 in range(4):
                        m = gu * 4 + m4
                        for kc in range(KC):
                            ins = nc.tensor.matmul(psall[:, (2 + gu) * 512 + m4 * 128: (2 + gu) * 512 + (m4 + 1) * 128],
                                                   lhsT=wgu[wi][:, kc, m * 128:(m + 1) * 128], rhs=xinT[i][:, kc, :],
                                                   start=(kc == 0), stop=(kc == KC - 1))
                    return ins
                k.op(pe, mmGU, R=[b_wgu[wi], b_xinT[i]], W=[psb[2 + gu]])
            k.op(act, lambda: nc.scalar.activation(out=sgt[i][:], in_=bank(2), func=AF.Silu), R=[psb[2]], W=[b_sgt[i]])
            k.op(dve, lambda: nc.vector.tensor_tensor(out=hmT[i][:].rearrange("p c t -> p (c t)"), in0=bank(3), in1=sgt[i][:], op=ALU.mult),
                 R=[psb[3], b_sgt[i]], W=[b_hmT[i]])

        def h_back(pos):
            b = border[pos]
            i = pos % 2
            wi = pos % NWB
            for n in range(4):
                def mmD(n=n, i=i, wi=wi):
                    ins = None
                    for kc in range(4):
                        ins = nc.tensor.matmul(bank(4 + n), lhsT=hmT[i][:, kc, :], rhs=wdn[wi][:, kc, n * 512:(n + 1) * 512],
                                               start=(kc == 0), stop=(kc == 3))
                    return ins
                k.op(pe, mmD, R=[b_hmT[i], b_wdn[wi]], W=[psb[4 + n]])
                if n % 2 == 0:
                    k.op(act, lambda: nc.scalar.copy(out=yst[i][:, n * 512:(n + 1) * 512], in_=bank(4 + n)), R=[psb[4 + n]], W=[b_yst[i]])
                else:
                    k.op(dve, lambda: nc.vector.tensor_copy(out=yst[i][:, n * 512:(n + 1) * 512], in_=bank(4 + n)), R=[psb[4 + n]], W=[b_yst[i]])
            ys_tks.append(k.dma(k.q_sp, ys_d[b * 128:(b + 1) * 128, :], yst[i][:], R=[b_yst[i]]))

        h_loads(0)
        h_front(0)
        for pos in range(NBLK):
            if pos + 1 < NBLK:
                h_loads(pos + 1)
            h_mid(pos)
            if pos + 1 < NBLK:
                h_front(pos + 1)
            h_back(pos)
        k.barrier()

        A.ptr = H_KEEP
        g2bc = A.alloc("g2bc", [128, D])
        gfbc = A.alloc("gfbc", [128, D])
        b_c3 = Buf()
        k.dma(k.q_sp, g2bc[:], modvec[4].partition_broadcast(128), W=[b_c3])
        k.dma(k.q_sp, gfbc[:], gfin_d.partition_broadcast(128), W=[b_c3])
        x1c = [A.alloc(f"x1c{i}", [128, D]) for i in range(2)]
        y1 = [A.alloc(f"y1_{i}", [128, D]) for i in range(2)]
        y2 = [A.alloc(f"y2_{i}", [128, D]) for i in range(2)]
        ot = [A.alloc(f"ot{i}", [128, D]) for i in range(2)]
        junk3 = A.alloc("junk3", [128, D], BF16)
        ss3 = [A.alloc(f"ss3_{i}", [128, 1]) for i in range(2)]
        b_x1c, b_y1, b_y2, b_ot, b_ss3 = [[Buf(), Buf()] for _ in range(5)]
        b_junk3 = Buf()
        def loadsI(s):
            i = s % 2
            k.dma(k.q_sp, x1c[i][:], x1_d[s * 128:(s + 1) * 128, :], W=[b_x1c[i]])
            k.dma(k.q_pool, y1[i][:], ys_d[:, :], R=[b_g], W=[b_y1[i]],
                  indirect=dict(out_offset=None, in_offset=IOA(ap=desti[:, s, 0:1].bitcast(U32), axis=0)))
            k.dma(k.q_pool, y2[i][:], ys_d[:, :], R=[b_g], W=[b_y2[i]],
                  indirect=dict(out_offset=None, in_offset=IOA(ap=desti[:, s, 1:2].bitcast(U32), axis=0)))

        def computeI(s):
            i = s % 2
            k.op(act, lambda: nc.scalar.activation(out=y1[i][:], in_=y1[i][:], func=AF.Copy, scale=w12all[:, s, 0:1]),
                 R=[b_rt], W=[b_y1[i]])
            k.op(dve, lambda: nc.vector.scalar_tensor_tensor(out=y1[i][:], in0=y2[i][:], scalar=w12all[:, s, 1:2], in1=y1[i][:],
                                                             op0=ALU.mult, op1=ALU.add),
                 R=[b_y2[i], b_rt], W=[b_y1[i]])
            k.op(pool, lambda: nc.gpsimd.tensor_tensor(out=y1[i][:], in0=y1[i][:], in1=g2bc[:], op=ALU.mult), R=[b_c3], W=[b_y1[i]])
            k.op(pool, lambda: nc.gpsimd.tensor_tensor(out=x1c[i][:], in0=x1c[i][:], in1=y1[i][:], op=ALU.add), R=[b_y1[i]], W=[b_x1c[i]])
            k.op(act, lambda: nc.scalar.activation(out=junk3[:], in_=x1c[i][:], func=AF.Square, accum_out=ss3[i][:]),
                 R=[b_x1c[i]], W=[b_junk3, b_ss3[i]])
            k.op(act, lambda: nc.scalar.activation(out=ss3[i][:], in_=ss3[i][:], func=AF.Sqrt, scale=1.0 / D, bias=1e-6), W=[b_ss3[i]])
            k.op(dve, lambda: nc.vector.reciprocal(out=ss3[i][:], in_=ss3[i][:]), W=[b_ss3[i]])
            k.op(dve, lambda: nc.vector.scalar_tensor_tensor(out=ot[i][:], in0=x1c[i][:], scalar=ss3[i][:, 0:1], in1=gfbc[:],
                                                             op0=ALU.mult, op1=ALU.mult),
                 R=[b_x1c[i], b_ss3[i], b_c3], W=[b_ot[i]])
            k.dma(k.q_sp, out[s * 128:(s + 1) * 128, :], ot[i][:], R=[b_ot[i]])

        loadsI(0)
        for s in range(16):
            if s + 1 < 16:
                loadsI(s + 1)
            computeI(s)
        k.barrier()
    return nc


_CACHE = {}


def _w_in_cols():
    cols = []
    qcols = np.arange(1024)
    hl = qcols % 64
    qswap = qcols - hl + (hl + 32) % 64
    for c in range(8):
        cols.append(qcols[c * 128:(c + 1) * 128])
    for c in range(8):
        cols.append(qswap[c * 128:(c + 1) * 128])
    for g in range(2):
        kc_ = 1024 + g * 64 + np.arange(64)
        ks_ = 1024 + g * 64 + (np.arange(64) + 32) % 64
        cols.append(np.concatenate([kc_, kc_]))
        cols.append(np.concatenate([ks_, ks_]))
    for c in range(8):
        cols.append(1280 + np.arange(c * 128, (c + 1) * 128))
    for c in range(8):
        cols.append(2304 + np.arange(c * 128, (c + 1) * 128))
    cols.append(1152 + np.arange(128))
    return cols


def _host_inputs(inp):
    f32 = np.float32
    x = np.ascontiguousarray(inp["x"], dtype=f32)
    c = np.asarray(inp["c"], dtype=f32)
    pos = np.asarray(inp["positions"]).astype(np.int32)
    b_adaT = np.ascontiguousarray(np.asarray(inp["b_ada"], f32).reshape(96, 128).T)
    gvecT = np.ascontiguousarray(np.concatenate([
        np.asarray(inp["g_mix"], f32).reshape(KC, 128).T,
        np.asarray(inp["g_ffn"], f32).reshape(KC, 128).T,
        np.asarray(inp["b_out"], f32).reshape(KC, 128).T], axis=1))
    w_ada = np.ascontiguousarray(inp["w_ada"], dtype=f32)
    identf = np.eye(128, dtype=f32)
    cols = _w_in_cols()
    allc = np.concatenate(cols)
    w_in = np.asarray(inp["w_in"], f32)
    b_in = np.asarray(inp["b_in"], f32)
    wp = w_in[:, allc].reshape(KC, 128, 37, 128)
    w_in_c = np.ascontiguousarray(wp.transpose(2, 1, 0, 3).reshape(37, 128, D))
    b_inT = np.ascontiguousarray(b_in[allc[:36 * 128]].reshape(36, 128).T)
    bv = np.ascontiguousarray(b_in[1152:1280])
    p = np.arange(128)
    invf32 = (f32(10000.0) ** (-np.arange(32, dtype=f32) * f32(2.0) / f32(64))).astype(f32)
    q_ = np.arange(128)[:, None]
    j_ = np.arange(256)[None, :]
    valid = (j_ >= q_ + 1) & (j_ <= q_ + 128)
    mask_std = np.where(valid, 0.0, -1e30).astype(f32)
    mask_first = mask_std.copy()
    mask_first[:, :128] = -1e30
    conv_w = np.asarray(inp["conv_w"], f32)
    cw = np.ascontiguousarray(conv_w.T.reshape(8, 128, CSZ).transpose(1, 0, 2).reshape(128, 8 * CSZ))
    cvec = np.ascontiguousarray(np.concatenate([
        np.asarray(inp["conv_b"], f32).reshape(8, 128).T,
        np.asarray(inp["conv_ln_g"], f32).reshape(8, 128).T,
        np.asarray(inp["conv_ln_b"], f32).reshape(8, 128).T], axis=1))
    w_out = np.ascontiguousarray(inp["w_out"], dtype=f32)
    sinks = np.ascontiguousarray(np.asarray(inp["attn_sinks"], f32).reshape(4, 2, 2).transpose(0, 2, 1).reshape(16))
    w_rf = np.concatenate([np.asarray(inp["w_group_router"], f32), np.asarray(inp["w_expert_router"], f32)], axis=1)
    w_r = np.ascontiguousarray(w_rf.reshape(KC, 128, 72).transpose(1, 0, 2).reshape(128, KC * 72))
    b_r = np.ascontiguousarray(np.concatenate([np.asarray(inp["b_group_router"], f32), np.asarray(inp["b_expert_router"], f32)]))
    g_final = np.ascontiguousarray(inp["g_final"], dtype=f32)
    iotas = np.zeros((128, 96 + 16 + 4), f32)
    iotas[:, 0:96] = np.arange(96, dtype=f32)[None, :]
    iotas[:, 96:112] = (np.arange(16)[None, :] * 128 + p[:, None]).astype(f32)
    iotas[:, 112:116] = (np.arange(4)[None, :] * 128 + p[:, None]).astype(f32)
    utri = (p[:, None] < p[None, :]).astype(f32)
    w_gu = np.ascontiguousarray(inp["w_gate_up"], dtype=f32).reshape(NE * D, 2 * FF)
    w_dn = np.ascontiguousarray(inp["w_down"], dtype=f32).reshape(NE * FF, D)
    maps = []
    for core in range(NCORES):
        b = core // 4
        s0 = (core % 4) * NT
        first = (s0 == 0)
        xh = np.zeros((NTT, D), f32)
        ph = np.zeros((NTT,), np.int32)
        if not first:
            xh[:NH] = x[b, s0 - NH:s0]
            ph[:NH] = pos[b, s0 - NH:s0]
        xh[NH:] = x[b, s0:s0 + NT]
        ph[NH:] = pos[b, s0:s0 + NT]
        smallc = np.zeros((128, 4), f32)
        smallc[:, 0] = invf32[p % 32]
        smallc[:, 1] = np.where(p % 64 < 32, -1.0, 1.0)
        smallc[:, 2] = 0.0 if first else 1.0
        masks = np.stack([mask_std, mask_first if first else mask_std], 0)
        m = {
            "xh": xh,
            "cT": np.ascontiguousarray(c[b].reshape(KC, 128).T),
            "w_ada": w_ada,
            "b_adaT": b_adaT,
            "gvecT": gvecT,
            "identf": identf,
            "w_in_c": w_in_c,
            "b_inT": b_inT,
            "bv": bv,
            "pos": ph,
            "smallc": smallc,
            "masks": np.ascontiguousarray(masks),
            "sinks": sinks,
            "cw": cw,
            "cvec": cvec,
            "w_out": w_out,
            "w_r": w_r,
            "b_r": b_r,
            "g_final": g_final,
            "iotas": iotas,
            "utri": utri,
            "w_gu": w_gu,
            "w_dn": w_dn,
        }
        maps.append(m)
    return maps


def kernel(**inputs):
    if "nc" not in _CACHE:
        _CACHE["nc"] = build_program()
    nc = _CACHE["nc"]
    maps = _host_inputs(inputs)
    res = run_bass_kernel_spmd(nc, maps, core_ids=list(range(NCORES)))
    outs = [np.asarray(r["out"]) for r in res.results]
    y = np.stack(outs, 0).reshape(2, 4 * NT, D).astype(np.float32)
    return y
```

```python
import numpy as np
from contextlib import ExitStack
import concourse.bass as bass
import concourse.mybir as mybir
from concourse.bass_utils import run_bass_kernel_spmd

F32 = mybir.dt.float32
BF16 = mybir.dt.bfloat16
I32 = mybir.dt.int32
U32 = mybir.dt.uint32
AF = mybir.ActivationFunctionType
ALU = mybir.AluOpType
AX = mybir.AxisListType

NCORES = 8
D = 2048
KC = 16
NT = 2048
NH = 128
NTT = NT + NH
TT = NTT // 128
NQ = 16
DH = 64
CONVC = 1024
CSZ = 31
NE = 64
FF = 512
NBLK = 96
SEM_LIMIT = 30000


class Eng:
    def __init__(self, k, eng, name):
        self.k = k
        self.eng = eng
        self.name = name
        self.nsem = 0
        self.new_sem()
        self.waited = {}

    def new_sem(self):
        self.sem = self.k.es.enter_context(self.k.nc.semaphore(f"s_{self.name}_{self.nsem}"))
        self.nsem += 1
        self.cnt = 0

    def wait(self, tk):
        if tk is None:
            return
        sem, val = tk[0], tk[1]
        key = id(sem)
        if self.waited.get(key, 0) >= val:
            return
        self.eng.wait_ge(sem, val)
        self.waited[key] = val

    def done(self, ins):
        if self.cnt >= SEM_LIMIT:
            self.new_sem()
        self.cnt += 1
        ins.then_inc(self.sem, 1)
        return (self.sem, self.cnt, False)


class Buf:
    def __init__(self, name=""):
        self.name = name
        self.ws = []
        self.readers = []


class DmaQ:
    def __init__(self, k, e, nsem, name):
        self.k = k
        self.e = e
        self.sems = [k.es.enter_context(k.nc.semaphore(f"d_{name}_{i}")) for i in range(nsem)]
        self.cnt = [0] * nsem
        self.i = 0

    def next(self):
        i = self.i
        self.i = (self.i + 1) % len(self.sems)
        if self.cnt[i] > 0:
            self.e.wait((self.sems[i], 16 * self.cnt[i]))
        self.cnt[i] += 1
        return self.sems[i], 16 * self.cnt[i]


class K:
    def __init__(self, nc, es):
        self.nc = nc
        self.es = es
        self.pe = Eng(self, nc.tensor, "pe")
        self.act = Eng(self, nc.scalar, "act")
        self.dve = Eng(self, nc.vector, "dve")
        self.pool = Eng(self, nc.gpsimd, "pool")
        self.sp = Eng(self, nc.sync, "sp")
        self.engs = [self.pe, self.act, self.dve, self.pool, self.sp]
        self.q_sp = DmaQ(self, self.sp, 16, "sp")
        self.q_pool = DmaQ(self, self.pool, 16, "pool")
        self.q_act = DmaQ(self, self.act, 4, "act")

    def op(self, e, fn, R=(), W=()):
        for b in R:
            for t in b.ws:
                e.wait(t)
        for b in W:
            for t in b.ws:
                e.wait(t)
            for r in b.readers:
                e.wait(r)
        ins = fn()
        tk = e.done(ins)
        for b in R:
            b.readers.append(tk)
        for b in W:
            b.ws = [tk]
            b.readers = []
        return tk

    def dma(self, q, out, in_, R=(), W=(), indirect=None, **kw):
        e = q.e
        for b in R:
            for t in b.ws:
                e.wait(t)
        app = []
        for b in W:
            a = (len(b.ws) > 0 and not b.readers and all(t[2] for t in b.ws))
            app.append(a)
            if not a:
                for t in b.ws:
                    e.wait(t)
                for r in b.readers:
                    e.wait(r)
        sem, val = q.next()
        if indirect is None:
            ins = e.eng.dma_start(out=out, in_=in_, **kw)
        else:
            ins = e.eng.indirect_dma_start(out=out, in_=in_, **indirect)
        ins.then_inc(sem, 16)
        tk = (sem, val, True)
        for b in R:
            b.readers.append(tk)
        for b, a in zip(W, app):
            if a:
                b.ws.append(tk)
            else:
                b.ws = [tk]
                b.readers = []
        return tk

    def barrier(self):
        tks = [(e.sem, e.cnt) for e in self.engs if e.cnt > 0]
        for q in (self.q_sp, self.q_pool, self.q_act):
            for s, c in zip(q.sems, q.cnt):
                if c > 0:
                    tks.append((s, 16 * c))
        for e in self.engs:
            for tk in tks:
                if tk[0] is e.sem:
                    continue
                e.wait(tk)


SB_BASE = 17408
SB_LIMIT = 228352


class Arena:
    def __init__(self, nc):
        self.nc = nc
        self.ptr = SB_BASE
        self.top = SB_LIMIT
        self.n = 0

    def alloc(self, name, shape, dt=F32):
        sz = int(np.prod(shape[1:])) * (2 if dt == BF16 else 4)
        sz = (sz + 31) // 32 * 32
        off = self.ptr
        self.ptr += sz
        assert self.ptr <= self.top, f"SBUF overflow at {name}: {self.ptr} > {self.top}"
        self.n += 1
        return self.nc.alloc_sbuf_tensor_at(f"{name}_{self.n}", list(shape), dt, offset=off)

    def alloc_top(self, name, shape, dt=F32):
        sz = int(np.prod(shape[1:])) * (2 if dt == BF16 else 4)
        sz = (sz + 31) // 32 * 32
        self.top -= sz
        assert self.ptr <= self.top, f"SBUF overflow at {name}"
        self.n += 1
        return self.nc.alloc_sbuf_tensor_at(f"{name}_{self.n}", list(shape), dt, offset=self.top)


PI = float(np.pi)
import os as _os
CSTOP = int(_os.environ.get('CSTOP', '0'))


def build_program(debug=None):
    nc = bass.Bass("TRN2", target_bir_lowering=False)

    def din(name, shape, dt=F32):
        return nc.dram_tensor(name, list(shape), dt, kind="ExternalInput").ap()

    def dscratch(name, shape, dt=F32):
        return nc.dram_tensor(name, list(shape), dt, kind="Internal").ap()

    xh = din("xh", [NTT, D])
    cT = din("cT", [128, KC])
    w_ada = din("w_ada", [D, 6 * D])
    b_adaT = din("b_adaT", [128, 96])
    gvecT = din("gvecT", [128, 3 * KC])
    identf_d = din("identf", [128, 128])
    w_in_c = din("w_in_c", [37, 128, D])
    b_inT = din("b_inT", [128, 36])
    bv_d = din("bv", [128])
    pos_d = din("pos", [NTT], I32)
    smallc = din("smallc", [128, 4])
    masks_d = din("masks", [2, 128, 256])
    sinks_d = din("sinks", [NQ])
    cw_d = din("cw", [128, 8 * CSZ])
    cvec_d = din("cvec", [128, 24])
    w_out_d = din("w_out", [D, D])
    w_r_d = din("w_r", [128, KC * 72])
    b_r_d = din("b_r", [72])
    gfin_d = din("g_final", [D])
    iota_d = din("iotas", [128, 96 + 16 + 4])
    utri_d = din("utri", [128, 128])
    w_gu_d = din("w_gu", [NE * 8 * 128, 2 * 2 * FF])
    w_dn_d = din("w_dn", [NE * 4 * 128, D])
    out = nc.dram_tensor("out", [NT, D], F32, kind="ExternalOutput").ap()
    h2_d = dscratch("h2_d", [NT, D], BF16)
    xin_d = dscratch("xin_d", [NBLK * 128, D], BF16)
    ys_d = dscratch("ys_d", [NBLK * 128, D])
    modvec = dscratch("modvec", [5, D])
    x1_d = dscratch("x1_d", [NT, D])
    if debug:
        dbg_q = nc.dram_tensor("dbg_q", [128, 8 * NT], BF16, kind="ExternalOutput").ap()
        dbg_k = nc.dram_tensor("dbg_k", [128, 2 * NTT], BF16, kind="ExternalOutput").ap()
        dbg_cat = nc.dram_tensor("dbg_cat", [128, 16 * NT], BF16, kind="ExternalOutput").ap()
        dbg_mod = nc.dram_tensor("dbg_mod", [128, 96], F32, kind="ExternalOutput").ap()
        dbg_rt = nc.dram_tensor("dbg_rt", [128, 16 * 132], F32, kind="ExternalOutput").ap()
        dbg_g = nc.dram_tensor("dbg_g", [128, 64 + 96 + 32], F32, kind="ExternalOutput").ap()

    with ExitStack() as es:
        k = K(nc, es)
        pe, act, dve, pool, sp = k.pe, k.act, k.dve, k.pool, k.sp
        A = Arena(nc)
        psall = nc.alloc_psum_tensor("psall", [128, 4096], F32)
        psb = [Buf(f"ps{i}") for i in range(8)]

        def bank(i, n=1):
            return psall[:, i * 512:(i + n) * 512]

        def bank_bf(i):
            return psall[:, i * 512:(i + 1) * 512].bitcast(BF16)

        ident_f = A.alloc("ident_f", [128, 128])
        ident_bf = A.alloc("ident_bf", [128, 128], BF16)
        ones_f = A.alloc("ones_f", [128, 128])
        modT = A.alloc("modT", [128, 96])
        gs1T = A.alloc("gs1T", [128, KC])
        gvT = A.alloc("gvT", [128, 3 * KC])
        smc = A.alloc("smc", [128, 4])
        sink_bc = A.alloc("sink_bc", [128, NQ])
        negsink = A.alloc("negsink", [128, NQ])
        bvbc = A.alloc("bvbc", [128, 128])
        binT = A.alloc("binT", [128, 36])
        cvec = A.alloc("cvec", [128, 24])
        cw = A.alloc("cw", [128, 8 * CSZ])
        mask_s = A.alloc("mask_s", [128, 256])
        mask_f = A.alloc("mask_f", [128, 256])
        mvf = A.alloc("mvf", [128, 80])
        mvs = A.alloc("mvs", [128, 128])
        b_const = Buf("const")
        for (dst, src) in ((ident_f[:], identf_d[:, :]), (gvT[:], gvecT[:, :]), (smc[:], smallc[:, :]),
                           (sink_bc[:], sinks_d.partition_broadcast(128)), (bvbc[:], bv_d.partition_broadcast(128)),
                           (binT[:], b_inT[:, :]), (cvec[:], cvec_d[:, :]), (cw[:], cw_d[:, :]),
                           (mask_s[:], masks_d[0]), (mask_f[:], masks_d[1])):
            k.dma(k.q_sp, dst, src, W=[b_const])
        k.op(dve, lambda: nc.vector.tensor_copy(out=ident_bf[:], in_=ident_f[:]), R=[b_const], W=[b_const])
        k.op(dve, lambda: nc.vector.memset(ones_f[:], 1.0), W=[b_const])
        k.op(dve, lambda: nc.vector.tensor_scalar(out=negsink[:], in0=sink_bc[:], scalar1=-1.0, scalar2=None, op0=ALU.mult),
             W=[b_const])
        invf = smc[:, 0:1]
        sgn = smc[:, 1:2]
        flag = smc[:, 2:3]
        P_END = A.ptr

        b_mod = Buf("mod")
        cT_s = A.alloc("cT_s", [128, KC])
        sT = A.alloc("sT", [128, KC], BF16)
        badaT = A.alloc("badaT", [128, 96])
        wada = [A.alloc(f"wada{i}", [128, 6 * D], BF16) for i in range(2)]
        b_w = [Buf("wada0"), Buf("wada1")]
        b_c = Buf("c")
        k.dma(k.q_sp, cT_s[:], cT[:, :], W=[b_c])
        k.dma(k.q_sp, badaT[:], b_adaT[:, :], W=[b_c])
        k.op(act, lambda: nc.scalar.activation(out=sT[:], in_=cT_s[:], func=AF.Silu), R=[b_c], W=[b_c])
        for kc in range(KC):
            w = wada[kc % 2]
            bw = b_w[kc % 2]
            for half in range(2):
                k.dma(k.q_pool, w[:, half * 6144:(half + 1) * 6144],
                      w_ada[kc * 128:(kc + 1) * 128, half * 6144:(half + 1) * 6144], W=[bw])

            def mm(kc=kc, w=w):
                ins = None
                for j in range(96):
                    ins = nc.tensor.matmul(psall[:, j:j + 1], lhsT=w[:, j * 128:(j + 1) * 128],
                                           rhs=sT[:, kc:kc + 1], start=(kc == 0 and j == 0),
                                           stop=(kc == KC - 1), skip_group_check=True)
                return ins
            k.op(pe, mm, R=[bw, b_c], W=[psb[0]])
        k.op(dve, lambda: nc.vector.tensor_tensor(out=modT[:], in0=psall[:, 0:96], in1=badaT[:], op=ALU.add),
             R=[psb[0], b_c], W=[b_mod])
        k.op(dve, lambda: nc.vector.scalar_tensor_tensor(out=gs1T[:], in0=modT[:, 16:32], scalar=1.0,
                                                         in1=gvT[:, 0:KC], op0=ALU.add, op1=ALU.mult),
             R=[b_mod, b_const], W=[b_mod])
        b_mv = Buf("mv")
        k.op(dve, lambda: nc.vector.tensor_copy(out=mvf[:, 0:16], in_=modT[:, 32:48]), R=[b_mod], W=[b_mv])
        k.op(dve, lambda: nc.vector.tensor_tensor(out=mvf[:, 16:32], in0=modT[:, 32:48], in1=gvT[:, 32:48], op=ALU.mult),
             R=[b_mod, b_const], W=[b_mv])
        k.op(dve, lambda: nc.vector.scalar_tensor_tensor(out=mvf[:, 32:48], in0=modT[:, 64:80], scalar=1.0,
                                                         in1=gvT[:, 16:32], op0=ALU.add, op1=ALU.mult),
             R=[b_mod, b_const], W=[b_mv])
        k.op(dve, lambda: nc.vector.tensor_copy(out=mvf[:, 48:64], in_=modT[:, 48:64]), R=[b_mod], W=[b_mv])
        k.op(dve, lambda: nc.vector.tensor_copy(out=mvf[:, 64:80], in_=modT[:, 80:96]), R=[b_mod], W=[b_mv])
        k.op(pe, lambda: nc.tensor.transpose(psall[0:80, 512:640], mvf[:, 0:80], ident_f[:]), R=[b_mv, b_const], W=[psb[1]])
        k.op(dve, lambda: nc.vector.tensor_copy(out=mvs[0:80, :], in_=psall[0:80, 512:640]), R=[psb[1]], W=[b_mv])
        k.dma(k.q_sp, modvec.rearrange("v (j p) -> (v j) p", p=128), mvs[0:80, :], R=[b_mv])
        if debug:
            k.dma(k.q_sp, dbg_mod[:, :], modT[:], R=[b_mod])
        k.barrier()
        A.ptr = P_END

        uT = A.alloc("uT", [128, 8, NTT], BF16)
        qT = A.alloc("qT", [128, 8, NT], BF16)
        Kd = [A.alloc(f"Kd{g}", [128, NTT], BF16) for g in range(2)]
        V = A.alloc("V", [128, TT, 128], BF16)
        b_uT, b_qT, b_Kd, b_V = Buf("uT"), Buf("qT"), Buf("Kd"), Buf("V")
        M1 = A.ptr

        cosT = A.alloc("cosT", [128, NTT])
        sinT = A.alloc("sinT", [128, NTT])
        b_tab = Buf("tab")
        M2 = A.ptr
        posi = A.alloc("posi", [128, NTT], I32)
        ang = A.alloc("ang", [128, NTT])
        tq = A.alloc("tq", [128, NTT])
        C1 = 6.28125
        C2 = 2 * PI - C1
        k.dma(k.q_sp, posi[:], pos_d.partition_broadcast(128), W=[b_tab])
        k.op(dve, lambda: nc.vector.tensor_copy(out=ang[:], in_=posi[:]), W=[b_tab])
        k.op(dve, lambda: nc.vector.tensor_scalar(out=ang[:], in0=ang[:], scalar1=invf, scalar2=None, op0=ALU.mult),
             R=[b_const], W=[b_tab])
        for (dst, off) in ((sinT, 0.0), (cosT, 0.5 * PI)):
            k.op(dve, lambda: nc.vector.tensor_scalar(out=dst[:], in0=ang[:], scalar1=off, scalar2=None, op0=ALU.add), W=[b_tab])
            k.op(dve, lambda: nc.vector.tensor_scalar(out=tq[:], in0=dst[:], scalar1=1.0 / (2 * PI), scalar2=0.5,
                                                      op0=ALU.mult, op1=ALU.add), W=[b_tab])
            k.op(dve, lambda: nc.vector.tensor_copy(out=posi[:], in_=tq[:]), W=[b_tab])
            k.op(dve, lambda: nc.vector.tensor_copy(out=tq[:], in_=posi[:]), W=[b_tab])
            k.op(dve, lambda: nc.vector.scalar_tensor_tensor(out=dst[:], in0=tq[:], scalar=-C1, in1=dst[:],
                                                             op0=ALU.mult, op1=ALU.add), W=[b_tab])
            k.op(dve, lambda: nc.vector.scalar_tensor_tensor(out=dst[:], in0=tq[:], scalar=-C2, in1=dst[:],
                                                             op0=ALU.mult, op1=ALU.add), W=[b_tab])
            k.op(dve, lambda: nc.vector.tensor_scalar(out=tq[:], in0=dst[:], scalar1=-PI, scalar2=None, op0=ALU.is_lt), W=[b_tab])
            k.op(dve, lambda: nc.vector.scalar_tensor_tensor(out=dst[:], in0=tq[:], scalar=2 * PI, in1=dst[:],
                                                             op0=ALU.mult, op1=ALU.add), W=[b_tab])
            k.op(dve, lambda: nc.vector.tensor_scalar(out=tq[:], in0=dst[:], scalar1=PI, scalar2=None, op0=ALU.is_gt), W=[b_tab])
            k.op(dve, lambda: nc.vector.scalar_tensor_tensor(out=dst[:], in0=tq[:], scalar=-2 * PI, in1=dst[:],
                                                             op0=ALU.mult, op1=ALU.add), W=[b_tab])
            k.op(dve, lambda: nc.vector.tensor_scalar(out=dst[:], in0=dst[:], scalar1=-3.14159, scalar2=3.14159,
                                                      op0=ALU.max, op1=ALU.min), W=[b_tab])
            k.op(act, lambda: nc.scalar.activation(out=dst[:], in_=dst[:], func=AF.Sin), W=[b_tab])
        k.op(dve, lambda: nc.vector.tensor_scalar(out=sinT[:], in0=sinT[:], scalar1=sgn, scalar2=None, op0=ALU.mult),
             R=[b_const], W=[b_tab])
        k.barrier()
        A.ptr = M2
        if debug == 'T':
            return nc

        HTOK = 1152
        hT = A.alloc("hT", [128, KC, HTOK], BF16)
        b_hT = Buf("hT")
        xt = [A.alloc(f"xt{i}", [128, D]) for i in range(2)]
        b_xt = [Buf(), Buf()]
        xs = [A.alloc(f"xs{i}", [128, D], BF16) for i in range(2)]
        b_xs = [Buf(), Buf()]
        junk = A.alloc("junkA", [128, D], BF16)
        b_junk = Buf()
        ssq = [A.alloc(f"ssq{i}", [128, 1]) for i in range(2)]
        b_ss = [Buf(), Buf()]
        wch = [A.alloc(f"wch{i}", [128, 2, D], BF16) for i in range(2)]
        b_wch = [Buf(), Buf()]
        tmp1 = [A.alloc(f"tmp1_{i}", [128, 512]) for i in range(2)]
        tmp2 = [A.alloc(f"tmp2_{i}", [128, 512]) for i in range(2)]
        b_tmp = [Buf(), Buf()]
        wv = A.alloc("wv", [128, D], BF16)
        b_wv = Buf()
        pair_ctr = [0]
        halves = [(0, 9, [(0, 128), (128, 640), (640, 1152)]), (9, 17, [(1152, 1664), (1664, 2176)])]
        pairs = [(c, 8 + c, 'q', c) for c in range(8)] + [(16, 17, 'k', 0), (18, 19, 'k', 1)] + \
                [(20 + c, 28 + c, 'glu', c) for c in range(8)]
        for (t_lo, t_hi, groups) in halves:
            tok0 = t_lo * 128
            for t in range(t_lo, t_hi):
                i = t % 2
                lt = (t - t_lo) * 128
                k.dma(k.q_sp, xt[i][:], xh[t * 128:(t + 1) * 128, :], W=[b_xt[i]])
                k.op(act, lambda: nc.scalar.activation(out=junk[:], in_=xt[i][:], func=AF.Square, accum_out=ssq[i][:]),
                     R=[b_xt[i]], W=[b_junk, b_ss[i]])
                k.op(act, lambda: nc.scalar.activation(out=ssq[i][:], in_=ssq[i][:], func=AF.Sqrt, scale=1.0 / D, bias=1e-6),
                     W=[b_ss[i]])
                k.op(dve, lambda: nc.vector.reciprocal(out=ssq[i][:], in_=ssq[i][:]), W=[b_ss[i]])
                k.op(act, lambda: nc.scalar.activation(out=xs[i][:], in_=xt[i][:], func=AF.Copy, scale=ssq[i][:, 0:1]),
                     R=[b_xt[i], b_ss[i]], W=[b_xs[i]])
                for hf in range(2):
                    pb = 6 + hf
                    pv = bank_bf(pb)

                    def tr(hf=hf, pv=pv, i=i):
                        ins = None
                        for j in range(8):
                            c = hf * 8 + j
                            ins = nc.tensor.transpose(pv[:, j * 128:(j + 1) * 128], xs[i][:, c * 128:(c + 1) * 128], ident_bf[:])
                        return ins
                    k.op(pe, tr, R=[b_xs[i], b_const], W=[psb[pb]])
                    for j in range(8):
                        c = hf * 8 + j
                        k.op(dve, lambda: nc.vector.tensor_scalar(out=hT[:, c, lt:lt + 128],
                                                                  in0=pv[:, j * 128:(j + 1) * 128],
                                                                  scalar1=gs1T[:, c:c + 1], scalar2=modT[:, c:c + 1],
                                                                  op0=ALU.mult, op1=ALU.add),
                             R=[psb[pb], b_mod], W=[b_hT])
            for (chA, chB, kind, c) in pairs:
                pi = pair_ctr[0] % 2
                pair_ctr[0] += 1
                w2 = wch[pi]
                k.dma(k.q_pool, w2[:, 0, :], w_in_c[chA], W=[b_wch[pi]])
                k.dma(k.q_pool, w2[:, 1, :], w_in_c[chB], W=[b_wch[pi]])
                for (g0, g1) in groups:
                    if kind == 'q' and g0 == 0:
                        continue
                    n = g1 - g0
                    l0 = g0 - tok0
                    pa, pb_ = (0, 1) if (pair_ctr[0] + g0 // 512) % 2 == 0 else (2, 3)
                    pair_ctr[0] += 0
                    for (which, pbank) in ((0, pa), (1, pb_)):
                        def mm(which=which, pbank=pbank, n=n, l0=l0, w2=w2):
                            ins = None
                            for kc in range(KC):
                                ins = nc.tensor.matmul(bank(pbank)[:, 0:n], lhsT=w2[:, which, kc * 128:(kc + 1) * 128],
                                                       rhs=hT[:, kc, l0:l0 + n], start=(kc == 0), stop=(kc == KC - 1))
                            return ins
                        k.op(pe, mm, R=[b_wch[pi], b_hT], W=[psb[pbank]])
                    ti = (g0 // 512) % 2
                    PA = bank(pa)[:, 0:n]
                    PB = bank(pb_)[:, 0:n]
                    if kind in ('q', 'k'):
                        k.op(dve, lambda: nc.vector.scalar_tensor_tensor(out=tmp1[ti][:, 0:n], in0=PA, scalar=binT[:, chA:chA + 1],
                                                                         in1=cosT[:, g0:g1], op0=ALU.add, op1=ALU.mult),
                             R=[psb[pa], b_const, b_tab], W=[b_tmp[ti]])
                        k.op(dve, lambda: nc.vector.scalar_tensor_tensor(out=tmp2[ti][:, 0:n], in0=PB, scalar=binT[:, chB:chB + 1],
                                                                         in1=sinT[:, g0:g1], op0=ALU.add, op1=ALU.mult),
                             R=[psb[pb_], b_const, b_tab], W=[b_tmp[ti]])
                        if kind == 'q':
                            dst = qT[:, c, g0 - NH:g1 - NH]
                            bdst = b_qT
                        else:
                            dst = Kd[c][:, g0:g1]
                            bdst = b_Kd
                        k.op(pool, lambda: nc.gpsimd.tensor_tensor(out=dst, in0=tmp1[ti][:, 0:n], in1=tmp2[ti][:, 0:n], op=ALU.add),
                             R=[b_tmp[ti]], W=[bdst])
                    else:
                        k.op(act, lambda: nc.scalar.activation(out=tmp2[ti][:, 0:n], in_=PB, func=AF.Sigmoid,
                                                               bias=binT[:, chB:chB + 1], scale=1.0),
                             R=[psb[pb_], b_const], W=[b_tmp[ti]])
                        if g0 == 0:
                            k.op(dve, lambda: nc.vector.scalar_tensor_tensor(out=tmp1[ti][:, 0:n], in0=PA, scalar=binT[:, chA:chA + 1],
                                                                             in1=tmp2[ti][:, 0:n], op0=ALU.add, op1=ALU.mult),
                                 R=[psb[pa], b_const], W=[b_tmp[ti]])
                            k.op(dve, lambda: nc.vector.tensor_scalar(out=uT[:, c, g0:g1], in0=tmp1[ti][:, 0:n], scalar1=flag,
                                                                      scalar2=None, op0=ALU.mult),
                                 R=[b_tmp[ti], b_const], W=[b_uT])
                        else:
                            k.op(dve, lambda: nc.vector.scalar_tensor_tensor(out=uT[:, c, g0:g1], in0=PA, scalar=binT[:, chA:chA + 1],
                                                                             in1=tmp2[ti][:, 0:n], op0=ALU.add, op1=ALU.mult),
                                 R=[psb[pa], b_const, b_tmp[ti]], W=[b_uT])
            k.dma(k.q_pool, wv[:], w_in_c[36], W=[b_wv])
            for t in range(t_lo, t_hi):
                lt = (t - t_lo) * 128
                pbank = 4 + (t % 2)

                def mmv(lt=lt, pbank=pbank):
                    ins = None
                    for kc in range(KC):
                        ins = nc.tensor.matmul(bank(pbank)[:, 0:128], lhsT=hT[:, kc, lt:lt + 128],
                                               rhs=wv[:, kc * 128:(kc + 1) * 128], start=(kc == 0), stop=(kc == KC - 1))
                    return ins
                k.op(pe, mmv, R=[b_wv, b_hT], W=[psb[pbank]])
                k.op(dve, lambda: nc.vector.tensor_tensor(out=V[:, t, :], in0=bank(pbank)[:, 0:128], in1=bvbc[:], op=ALU.add),
                     R=[psb[pbank], b_const], W=[b_V])
        if debug:
            k.dma(k.q_sp, dbg_q[:, :], qT[:].rearrange("p c t -> p (c t)"), R=[b_qT])
            k.dma(k.q_sp, dbg_k[:, 0:NTT], Kd[0][:], R=[b_Kd])
            k.dma(k.q_sp, dbg_k[:, NTT:2 * NTT], Kd[1][:], R=[b_Kd])
        k.barrier()
        A.ptr = M1
        if debug == 'B':
            return nc

        catA = A.alloc_top("catA", [128, 8, NT], BF16)
        catC = A.alloc_top("catC", [128, 8, NT], BF16)
        b_catA, b_catC = Buf("catA"), Buf("catC")
        NB4 = 4
        Sm = [A.alloc(f"Sm{i}", [128, 4, 256]) for i in range(NB4)]
        Pm = [A.alloc(f"Pm{i}", [128, 4, 256], BF16) for i in range(NB4)]
        PTs = [A.alloc(f"PTs{i}", [128, 1024], BF16) for i in range(NB4)]
        osb = [A.alloc(f"osb{i}", [128, NQ * DH], BF16) for i in range(2)]
        sm_small = [A.alloc(f"sms{i}", [128, 32]) for i in range(NB4)]
        b_Sm, b_Pm, b_PTs, b_sms = [[Buf() for _ in range(NB4)] for _ in range(4)]
        b_osb = [Buf(), Buf()]
        NIT = 64
        zt = A.alloc("zt", [128, D], BF16)
        b_zt = Buf()
        k.op(pool, lambda: nc.gpsimd.memset(zt[:], 0.0), W=[b_zt])
        zero_tks = []
        for n in range(NBLK):
            zero_tks.append(k.dma(k.q_sp, xin_d[n * 128:(n + 1) * 128, :], zt[:], R=[b_zt]))

        def smv(i):
            sm = sm_small[i]
            return (sm[:, 0:4], sm[:, 4:8], sm[:, 8:12], sm[:, 12:16], sm[:, 16:20], sm[:, 20:24], sm[:, 24:28])

        def stA(it):
            s, hg = it // 4, it % 4
            i = it % NB4
            g = hg // 2
            sb0 = 2 * (it % 2)
            mask = mask_f if s == 0 else mask_s
            Sps = bank(sb0, 2)

            def mmS():
                ins = None
                for hl in range(4):
                    h = hg * 4 + (0, 2, 1, 3)[hl]
                    c, half = h // 2, h % 2
                    ins = nc.tensor.matmul(psall[:, sb0 * 512 + hl * 256: sb0 * 512 + (hl + 1) * 256],
                                           lhsT=qT[half * 64:(half + 1) * 64, c, s * 128:(s + 1) * 128],
                                           rhs=Kd[g][half * 64:(half + 1) * 64, s * 128:s * 128 + 256],
                                           start=True, stop=True)
                return ins
            k.op(pe, mmS, R=[b_qT, b_Kd], W=[psb[sb0], psb[sb0 + 1]])
            rmax, negm, rsum, dlt, esink, den, rr = smv(i)
            k.op(dve, lambda: nc.vector.scalar_tensor_tensor(
                out=Sm[i][:], in0=Sps.rearrange("p (h k) -> p h k", h=4), scalar=0.125,
                in1=mask[:].unsqueeze(1).broadcast_to([128, 4, 256]), op0=ALU.mult, op1=ALU.add),
                R=[psb[sb0], psb[sb0 + 1], b_const], W=[b_Sm[i]])
            k.op(dve, lambda: nc.vector.tensor_reduce(out=rmax, in_=Sm[i][:], axis=AX.X, op=ALU.max),
                 R=[b_Sm[i]], W=[b_sms[i]])
            k.op(dve, lambda: nc.vector.scalar_tensor_tensor(out=negm, in0=rmax, scalar=-1.0,
                                                             in1=negsink[:, hg * 4:hg * 4 + 4], op0=ALU.mult, op1=ALU.min),
                 R=[b_const], W=[b_sms[i]])
            k.op(dve, lambda: nc.vector.tensor_tensor(out=dlt, in0=negm, in1=negsink[:, hg * 4:hg * 4 + 4], op=ALU.subtract),
                 R=[b_const], W=[b_sms[i]])

        def stB(it):
            i = it % NB4
            rmax, negm, rsum, dlt, esink, den, rr = smv(i)
            for hl in range(4):
                k.op(act, lambda: nc.scalar.activation(out=Pm[i][:, hl, :], in_=Sm[i][:, hl, :], func=AF.Exp,
                                                       bias=negm[:, hl:hl + 1], scale=1.0, accum_out=rsum[:, hl:hl + 1]),
                     R=[b_Sm[i]], W=[b_Pm[i], b_sms[i]])
            k.op(act, lambda: nc.scalar.activation(out=esink, in_=dlt, func=AF.Exp), W=[b_sms[i]])
            k.op(dve, lambda: nc.vector.tensor_tensor(out=den, in0=rsum, in1=esink, op=ALU.add), W=[b_sms[i]])
            k.op(dve, lambda: nc.vector.reciprocal(out=rr, in_=den), W=[b_sms[i]])

        def stC(it):
            i = it % NB4
            ptb = 4 + (it % 2)
            PTp = bank_bf(ptb)

            def trP():
                ins = None
                for hl in range(4):
                    for blk in range(2):
                        o = (hl * 2 + blk) * 128
                        ins = nc.tensor.transpose(PTp[:, o:o + 128], Pm[i][:, hl, blk * 128:(blk + 1) * 128], ident_bf[:])
                return ins
            k.op(pe, trP, R=[b_Pm[i], b_const], W=[psb[ptb]])
            k.op(act, lambda: nc.scalar.copy(out=PTs[i][:], in_=PTp), R=[psb[ptb]], W=[b_PTs[i]])

        def stD(it):
            s, hg = it // 4, it % 4
            i = it % NB4
            g = hg // 2
            oi = s % 2
            rmax, negm, rsum, dlt, esink, den, rr = smv(i)

            def mmO():
                ins = None
                for hl in range(4):
                    for blk in range(2):
                        o = (hl * 2 + blk) * 128
                        ins = nc.tensor.matmul(psall[:, 6 * 512 + hl * 64: 6 * 512 + (hl + 1) * 64],
                                               lhsT=PTs[i][:, o:o + 128], rhs=V[:, s + blk, g * 64:(g + 1) * 64],
                                               start=(blk == 0), stop=(blk == 1))
                return ins
            k.op(pe, mmO, R=[b_PTs[i], b_V], W=[psb[6]])
            k.op(dve, lambda: nc.vector.tensor_tensor(
                out=osb[oi][:, hg * 256:(hg + 1) * 256].rearrange("p (a b d) -> p b a d", a=2, b=2),
                in0=psall[:, 6 * 512: 6 * 512 + 256].rearrange("p (b a d) -> p b a d", a=2, b=2),
                in1=rr.rearrange("p (b a) -> p b a", a=2).unsqueeze(3).broadcast_to([128, 2, 2, 64]), op=ALU.mult),
                R=[psb[6], b_sms[i]], W=[b_osb[oi]])
            if hg == 3:
                oTp = bank_bf(7)

                def trO():
                    ins = None
                    for c in range(8):
                        ins = nc.tensor.transpose(oTp[:, c * 128:(c + 1) * 128], osb[oi][:, c * 128:(c + 1) * 128], ident_bf[:])
                    return ins
                k.op(pe, trO, R=[b_osb[oi], b_const], W=[psb[7]])
                k.op(act, lambda: nc.scalar.copy(out=catA[:, :, s * 128:(s + 1) * 128],
                                                 in_=oTp.rearrange("p (c t) -> p c t", c=8)),
                     R=[psb[7]], W=[b_catA])

        for step in range(NIT + 3):
            if step < NIT:
                stA(step)
            if 0 <= step - 1 < NIT:
                stB(step - 1)
            if 0 <= step - 2 < NIT:
                stC(step - 2)
            if 0 <= step - 3 < NIT:
                stD(step - 3)
        k.barrier()
        if debug == 'C':
            return nc

        A.ptr = M1 - (32768 + 2 * 4352 + 4352)
        diag = A.alloc("diag", [128, 8, CSZ, 128], BF16)
        b_diag = Buf()
        ybuf = A.alloc("ybuf", [128, 8, 512])
        b_y = Buf()
        ysq = [A.alloc(f"ysq{i}", [128, 512]) for i in range(2)]
        b_ysq = [Buf(), Buf()]
        mean = A.alloc("mean", [128, 512])
        rstd = A.alloc("rstd", [128, 512])
        msq = A.alloc("msq", [128, 512])
        b_st = Buf()
        t1 = [A.alloc(f"t1_{i}", [128, 512]) for i in range(2)]
        b_t1 = [Buf(), Buf()]
        n_d = 0
        for cc in range(8):
            for j in range(CSZ):
                if n_d % 2 == 0:
                    k.op(dve, lambda: nc.vector.tensor_scalar(out=diag[:, cc, j, :], in0=ident_f[:],
                                                              scalar1=cw[:, cc * CSZ + j: cc * CSZ + j + 1], scalar2=None, op0=ALU.mult),
                         R=[b_const], W=[b_diag])
                else:
                    k.op(pool, lambda: nc.gpsimd.tensor_scalar(out=diag[:, cc, j, :], in0=ident_f[:],
                                                               scalar1=cw[:, cc * CSZ + j: cc * CSZ + j + 1], scalar2=1.0,
                                                               op0=ALU.mult, op1=ALU.mult),
                         R=[b_const], W=[b_diag])
                n_d += 1
        for g in range(4):
            s0 = g * 512
            for cc in range(8):
                pbank = cc % 4

                def mmC(cc=cc, pbank=pbank, s0=s0):
                    ins = None
                    for j in range(CSZ):
                        o = NH + s0 - (CSZ - 1) + j
                        ins = nc.tensor.matmul(bank(pbank), lhsT=diag[:, cc, j, :], rhs=uT[:, cc, o:o + 512],
                                               start=(j == 0), stop=(j == CSZ - 1))
                    return ins
                k.op(pe, mmC, R=[b_diag, b_uT], W=[psb[pbank]])
                k.op(act, lambda: nc.scalar.activation(out=ybuf[:, cc, :], in_=bank(pbank), func=AF.Identity,
                                                       bias=cvec[:, cc:cc + 1], scale=1.0),
                     R=[psb[pbank], b_const], W=[b_y])
            def mmS1():
                ins = None
                for cc in range(8):
                    ins = nc.tensor.matmul(bank(4), lhsT=ones_f[:], rhs=ybuf[:, cc, :], start=(cc == 0), stop=(cc == 7))
                return ins
            k.op(pe, mmS1, R=[b_y, b_const], W=[psb[4]])
            for cc in range(8):
                i = cc % 2
                k.op(act, lambda: nc.scalar.activation(out=ysq[i][:], in_=ybuf[:, cc, :], func=AF.Square),
                     R=[b_y], W=[b_ysq[i]])
                k.op(pe, lambda: nc.tensor.matmul(bank(5), lhsT=ones_f[:], rhs=ysq[i][:], start=(cc == 0), stop=(cc == 7),
                                                  skip_group_check=True),
                     R=[b_ysq[i], b_const], W=[psb[5]])
            k.op(dve, lambda: nc.vector.tensor_scalar(out=mean[:], in0=bank(4), scalar1=1.0 / CONVC, scalar2=None, op0=ALU.mult),
                 R=[psb[4]], W=[b_st])
            k.op(dve, lambda: nc.vector.tensor_tensor(out=msq[:], in0=mean[:], in1=mean[:], op=ALU.mult), W=[b_st])
            k.op(dve, lambda: nc.vector.scalar_tensor_tensor(out=rstd[:], in0=bank(5), scalar=1.0 / CONVC, in1=msq[:],
                                                             op0=ALU.mult, op1=ALU.subtract),
                 R=[psb[5]], W=[b_st])
            k.op(act, lambda: nc.scalar.activation(out=rstd[:], in_=rstd[:], func=AF.Sqrt, bias=1e-5, scale=1.0), W=[b_st])
            k.op(dve, lambda: nc.vector.reciprocal(out=rstd[:], in_=rstd[:]), W=[b_st])
            for cc in range(8):
                i = cc % 2
                k.op(dve, lambda: nc.vector.tensor_tensor(out=t1[i][:], in0=ybuf[:, cc, :], in1=mean[:], op=ALU.subtract),
                     R=[b_y, b_st], W=[b_t1[i]])
                k.op(pool, lambda: nc.gpsimd.tensor_tensor(out=t1[i][:], in0=t1[i][:], in1=rstd[:], op=ALU.mult),
                     R=[b_st], W=[b_t1[i]])
                k.op(act, lambda: nc.scalar.activation(out=catC[:, cc, s0:s0 + 512], in_=t1[i][:], func=AF.Silu,
                                                       scale=cvec[:, 8 + cc:9 + cc], bias=cvec[:, 16 + cc:17 + cc]),
                     R=[b_t1[i], b_const], W=[b_catC])
        if debug:
            k.dma(k.q_sp, dbg_cat[:, 0:8 * NT], catA[:].rearrange("p c t -> p (c t)"), R=[b_catA])
            k.dma(k.q_sp, dbg_cat[:, 8 * NT:16 * NT], catC[:].rearrange("p c t -> p (c t)"), R=[b_catC])
        k.barrier()
        if debug == 'D':
            return nc

        A.ptr = P_END
        w_o = A.alloc("w_o", [128, KC, D], BF16)
        b_wo = Buf()
        g1bc = A.alloc("g1bc", [128, D])
        gbbc = A.alloc("gbbc", [128, D])
        b_bc = Buf()
        xt2 = [A.alloc(f"xt2_{i}", [128, D]) for i in range(2)]
        x1t = [A.alloc(f"x1t_{i}", [128, D]) for i in range(2)]
        b_xt2, b_x1t = [Buf(), Buf()], [Buf(), Buf()]
        for kc in range(KC):
            k.dma(k.q_pool, w_o[:, kc, :], w_out_d[kc * 128:(kc + 1) * 128, :], W=[b_wo])
        k.dma(k.q_sp, g1bc[:], modvec[0].partition_broadcast(128), W=[b_bc])
        k.dma(k.q_sp, gbbc[:], modvec[1].partition_broadcast(128), W=[b_bc])
        k.dma(k.q_sp, xt2[0][:], xh[NH: NH + 128, :], W=[b_xt2[0]])
        for s in range(16):
            i = s % 2
            if s + 1 < 16:
                k.dma(k.q_sp, xt2[1 - i][:], xh[NH + (s + 1) * 128: NH + (s + 2) * 128, :], W=[b_xt2[1 - i]])
            k.op(pool, lambda: nc.gpsimd.tensor_tensor(out=xt2[i][:], in0=xt2[i][:], in1=gbbc[:], op=ALU.add),
                 R=[b_bc], W=[b_xt2[i]])
            for n in range(4):
                pbank = 4 * i + n

                def mmE(n=n, pbank=pbank, s=s):
                    ins = None
                    for kc in range(KC):
                        src = catA if kc < 8 else catC
                        ins = nc.tensor.matmul(bank(pbank), lhsT=src[:, kc % 8, s * 128:(s + 1) * 128],
                                               rhs=w_o[:, kc, n * 512:(n + 1) * 512], start=(kc == 0), stop=(kc == KC - 1))
                    return ins
                k.op(pe, mmE, R=[b_catA, b_catC, b_wo], W=[psb[pbank]])
                k.op(dve, lambda: nc.vector.tensor_tensor(out=x1t[i][:, n * 512:(n + 1) * 512], in0=bank(pbank),
                                                          in1=g1bc[:, n * 512:(n + 1) * 512], op=ALU.mult),
                     R=[psb[pbank], b_bc], W=[b_x1t[i]])
            k.op(pool, lambda: nc.gpsimd.tensor_tensor(out=x1t[i][:], in0=x1t[i][:], in1=xt2[i][:], op=ALU.add),
                 R=[b_xt2[i]], W=[b_x1t[i]])
            k.dma(k.q_sp, x1_d[s * 128:(s + 1) * 128, :], x1t[i][:], R=[b_x1t[i]])
            if debug == 'E':
                k.dma(k.q_sp, out[s * 128:(s + 1) * 128, :], x1t[i][:], R=[b_x1t[i]])
        k.barrier()
        if debug == 'E':
            return nc

        A.ptr = P_END
        A.top = SB_LIMIT
        ones_bf = A.alloc("ones_bf", [128, 128], BF16)
        utri = A.alloc("utri", [128, 128], BF16)
        utri_f = A.alloc("utri_f", [128, 128])
        iot = A.alloc("iot", [128, 96 + 16 + 4])
        w12all = A.alloc("w12all", [128, 16, 2])
        destf = A.alloc("destf", [128, 16, 2])
        desti = A.alloc("desti", [128, 16, 2], I32)
        idx_gu = A.alloc("idx_gu", [128, NBLK, 8], I32)
        idx_dn = A.alloc("idx_dn", [128, NBLK, 4], I32)
        b_rt = Buf("routing")
        b_c2 = Buf("const2")
        k.dma(k.q_sp, utri_f[:], utri_d[:, :], W=[b_c2])
        k.dma(k.q_sp, iot[:], iota_d[:, :], W=[b_c2])
        k.op(dve, lambda: nc.vector.tensor_copy(out=utri[:], in_=utri_f[:]), W=[b_c2])
        k.op(dve, lambda: nc.vector.memset(ones_bf[:], 1.0), W=[b_c2])
        H_KEEP = A.ptr
        O1all = A.alloc("O1all", [128, 16, 64])
        O2all = A.alloc("O2all", [128, 16, 64])
        O12all = A.alloc("O12all", [128, 16, 64], BF16)
        G_KEEP = A.ptr
        gs2bc = A.alloc("gs2bc", [128, D])
        sh2bc = A.alloc("sh2bc", [128, D])
        w_r = A.alloc("w_r", [128, KC, 72])
        brbc = A.alloc("brbc", [128, 72])
        k.dma(k.q_sp, gs2bc[:], modvec[2].partition_broadcast(128), W=[b_c2])
        k.dma(k.q_sp, sh2bc[:], modvec[3].partition_broadcast(128), W=[b_c2])
        k.dma(k.q_sp, w_r[:].rearrange("p c n -> p (c n)"), w_r_d[:, :], W=[b_c2])
        k.dma(k.q_sp, brbc[:], b_r_d.partition_broadcast(128), W=[b_c2])
        x1b = [A.alloc(f"x1b{i}", [128, D]) for i in range(2)]
        h2f = [A.alloc(f"h2f{i}", [128, D]) for i in range(2)]
        h2b = [A.alloc(f"h2b{i}", [128, D], BF16) for i in range(2)]
        h2T = A.alloc("h2T", [128, KC, 128])
        lg_all = A.alloc("lg_all", [128, 16, 72])
        rs = [A.alloc(f"rs{i}", [128, 8]) for i in range(2)]
        junk2 = A.alloc("junk2", [128, D], BF16)
        b_x1b, b_h2f, b_h2b, b_rs = [Buf(), Buf()], [Buf(), Buf()], [Buf(), Buf()], [Buf(), Buf()]
        b_h2T, b_junk2, b_lg = Buf(), Buf(), Buf()
        def e2_stage1(s):
            i = s % 2
            ss2 = rs[i][:, 0:1]
            k.dma(k.q_sp, x1b[i][:], x1_d[s * 128:(s + 1) * 128, :], W=[b_x1b[i]])
            k.op(act, lambda: nc.scalar.activation(out=junk2[:], in_=x1b[i][:], func=AF.Square, accum_out=ss2),
                 R=[b_x1b[i]], W=[b_junk2, b_rs[i]])
            k.op(act, lambda: nc.scalar.activation(out=ss2, in_=ss2, func=AF.Sqrt, scale=1.0 / D, bias=1e-6), W=[b_rs[i]])
            k.op(dve, lambda: nc.vector.reciprocal(out=ss2, in_=ss2), W=[b_rs[i]])
            k.op(dve, lambda: nc.vector.scalar_tensor_tensor(out=h2f[i][:], in0=x1b[i][:], scalar=ss2, in1=gs2bc[:],
                                                             op0=ALU.mult, op1=ALU.mult),
                 R=[b_x1b[i], b_rs[i], b_c2], W=[b_h2f[i]])
            k.op(pool, lambda: nc.gpsimd.tensor_tensor(out=h2f[i][:], in0=h2f[i][:], in1=sh2bc[:], op=ALU.add),
                 R=[b_c2], W=[b_h2f[i]])
            k.op(act, lambda: nc.scalar.copy(out=h2b[i][:], in_=h2f[i][:]), R=[b_h2f[i]], W=[b_h2b[i]])
            k.dma(k.q_sp, h2_d[s * 128:(s + 1) * 128, :], h2b[i][:], R=[b_h2b[i]])

        def e2_stage2(s):
            i = s % 2
            hT_ = h2T2[i]
            for q4 in range(4):
                def trH(q4=q4, i=i):
                    ins = None
                    for j in range(4):
                        c = q4 * 4 + j
                        ins = nc.tensor.transpose(psall[:, q4 * 512 + j * 128: q4 * 512 + (j + 1) * 128],
                                                  h2f[i][:, c * 128:(c + 1) * 128], ident_f[:])
                    return ins
                k.op(pe, trH, R=[b_h2f[i], b_const], W=[psb[q4]])
                if q4 % 2 == 0:
                    k.op(act, lambda: nc.scalar.copy(out=hT_[:, q4 * 4:(q4 + 1) * 4, :].rearrange("p c t -> p (c t)"), in_=bank(q4)),
                         R=[psb[q4]], W=[b_h2T2[i]])
                else:
                    k.op(dve, lambda: nc.vector.tensor_copy(out=hT_[:, q4 * 4:(q4 + 1) * 4, :].rearrange("p c t -> p (c t)"), in_=bank(q4)),
                         R=[psb[q4]], W=[b_h2T2[i]])
            pbl = 4 + (s % 2)

            def mmR(pbl=pbl):
                ins = None
                for kc in range(KC):
                    ins = nc.tensor.matmul(psall[:, pbl * 512: pbl * 512 + 72], lhsT=hT_[:, kc, :], rhs=w_r[:, kc, :],
                                           start=(kc == 0), stop=(kc == KC - 1))
                return ins
            k.op(pe, mmR, R=[b_h2T2[i], b_c2], W=[psb[pbl]])
            k.op(dve, lambda: nc.vector.tensor_tensor(out=lg_all[:, s, :], in0=psall[:, pbl * 512: pbl * 512 + 72], in1=brbc[:], op=ALU.add),
                 R=[psb[pbl], b_c2], W=[b_lg])

        h2T2 = [h2T, A.alloc("h2Tb", [128, KC, 128])]
        b_h2T2 = [Buf(), Buf()]
        e2_stage1(0)
        for s in range(16):
            if s + 1 < 16:
                e2_stage1(s + 1)
            e2_stage2(s)
        T16 = 16
        gmax = A.alloc("gmax", [128, T16]); ohg = A.alloc("ohg", [128, T16, 8]); ege = A.alloc("ege", [128, T16, 8])
        gsum = A.alloc("gsum", [128, T16]); pg = A.alloc("pg", [128, T16]); selt = A.alloc("selt", [128, T16, 8, 8])
        eg = A.alloc("eg", [128, T16, 8]); v1 = A.alloc("v1", [128, T16]); oh1 = A.alloc("oh1", [128, T16, 8])
        eg2 = A.alloc("eg2", [128, T16, 8]); v2 = A.alloc("v2", [128, T16]); oh2 = A.alloc("oh2", [128, T16, 8])
        d21 = A.alloc("d21", [128, T16]); wA = A.alloc("wA", [128, T16])
        gl = lg_all[:, :, 0:8]
        el4 = lg_all[:, :, 8:72].rearrange("p t (g j) -> p t g j", g=8)
        Wr = [b_rt]

        def bc3(ap2):
            return ap2.unsqueeze(2).broadcast_to([128, T16, 8])
        k.op(dve, lambda: nc.vector.tensor_reduce(out=gmax[:], in_=gl, axis=AX.X, op=ALU.max), R=[b_lg], W=Wr)
        k.op(dve, lambda: nc.vector.tensor_tensor(out=ohg[:], in0=gl, in1=bc3(gmax[:]), op=ALU.is_equal), R=[b_lg], W=Wr)
        k.op(dve, lambda: nc.vector.tensor_tensor(out=ege[:], in0=gl, in1=bc3(gmax[:]), op=ALU.subtract), R=[b_lg], W=Wr)
        k.op(act, lambda: nc.scalar.activation(out=ege[:], in_=ege[:], func=AF.Exp), W=Wr)
        k.op(dve, lambda: nc.vector.tensor_reduce(out=gsum[:], in_=ege[:], axis=AX.X, op=ALU.add), W=Wr)
        k.op(dve, lambda: nc.vector.reciprocal(out=pg[:], in_=gsum[:]), W=Wr)
        k.op(dve, lambda: nc.vector.tensor_tensor(out=selt[:], in0=el4, in1=ohg[:].unsqueeze(3).broadcast_to([128, T16, 8, 8]),
                                                  op=ALU.mult), R=[b_lg], W=Wr)
        k.op(dve, lambda: nc.vector.tensor_reduce(out=eg[:], in_=selt[:].rearrange("p t g j -> p t j g"), axis=AX.X, op=ALU.add), W=Wr)
        k.op(dve, lambda: nc.vector.tensor_reduce(out=v1[:], in_=eg[:], axis=AX.X, op=ALU.max), W=Wr)
        k.op(dve, lambda: nc.vector.tensor_tensor(out=oh1[:], in0=eg[:], in1=bc3(v1[:]), op=ALU.is_equal), W=Wr)
        k.op(dve, lambda: nc.vector.scalar_tensor_tensor(out=eg2[:], in0=oh1[:], scalar=-1e30, in1=eg[:], op0=ALU.mult, op1=ALU.add), W=Wr)
        k.op(dve, lambda: nc.vector.tensor_reduce(out=v2[:], in_=eg2[:], axis=AX.X, op=ALU.max), W=Wr)
        k.op(dve, lambda: nc.vector.tensor_tensor(out=oh2[:], in0=eg2[:], in1=bc3(v2[:]), op=ALU.is_equal), W=Wr)
        k.op(dve, lambda: nc.vector.tensor_tensor(out=d21[:], in0=v2[:], in1=v1[:], op=ALU.subtract), W=Wr)
        k.op(act, lambda: nc.scalar.activation(out=d21[:], in_=d21[:], func=AF.Exp), W=Wr)
        k.op(dve, lambda: nc.vector.tensor_scalar(out=d21[:], in0=d21[:], scalar1=1.0, scalar2=None, op0=ALU.add), W=Wr)
        k.op(dve, lambda: nc.vector.reciprocal(out=wA[:], in_=d21[:]), W=Wr)
        k.op(dve, lambda: nc.vector.tensor_tensor(out=w12all[:, :, 0], in0=pg[:], in1=wA[:], op=ALU.mult), W=Wr)
        k.op(dve, lambda: nc.vector.tensor_tensor(out=w12all[:, :, 1], in0=pg[:], in1=w12all[:, :, 0], op=ALU.subtract), W=Wr)
        O1v = O1all[:].rearrange("p t (g j) -> p t g j", g=8)
        O2v = O2all[:].rearrange("p t (g j) -> p t g j", g=8)
        k.op(dve, lambda: nc.vector.tensor_tensor(out=O1v, in0=ohg[:].unsqueeze(3).broadcast_to([128, T16, 8, 8]),
                                                  in1=oh1[:].unsqueeze(2).broadcast_to([128, T16, 8, 8]), op=ALU.mult), W=Wr)
        k.op(dve, lambda: nc.vector.tensor_tensor(out=O2v, in0=ohg[:].unsqueeze(3).broadcast_to([128, T16, 8, 8]),
                                                  in1=oh2[:].unsqueeze(2).broadcast_to([128, T16, 8, 8]), op=ALU.mult), W=Wr)
        k.op(dve, lambda: nc.vector.tensor_tensor(out=O12all[:], in0=O1all[:], in1=O2all[:], op=ALU.add), W=Wr)
        k.barrier()

        A.ptr = G_KEEP
        cnt = A.alloc("cnt", [128, 64])
        nbk = A.alloc("nbk", [128, 64])
        cs = [A.alloc(f"cs{i}", [128, 64]) for i in range(2)]
        pst = A.alloc("pst", [128, 64])
        bef = A.alloc("bef", [128, NBLK])
        cmp3 = A.alloc("cmp3", [128, NBLK, 64])
        tf1 = A.alloc("tf1", [128, NBLK, 8])
        b_g = Buf("g")

        def mmCnt():
            ins = None
            for s in range(16):
                ins = nc.tensor.matmul(psall[:, 0:64], lhsT=ones_bf[:], rhs=O12all[:, s, :], start=(s == 0), stop=(s == 15))
            return ins
        k.op(pe, mmCnt, R=[b_rt, b_c2], W=[psb[0]])
        k.op(dve, lambda: nc.vector.tensor_copy(out=cnt[:], in_=psall[:, 0:64]), R=[psb[0]], W=[b_g])
        k.op(dve, lambda: nc.vector.memset(nbk[:], 0.0), W=[b_g])
        for m in range(16):
            k.op(dve, lambda: nc.vector.scalar_tensor_tensor(out=nbk[:], in0=cnt[:], scalar=128.0 * m + 0.5, in1=nbk[:],
                                                             op0=ALU.is_gt, op1=ALU.add), W=[b_g])
        k.op(dve, lambda: nc.vector.tensor_copy(out=cs[0][:], in_=nbk[:]), W=[b_g])
        cur = 0
        for dsh in (1, 2, 4, 8, 16, 32):
            a_, b_ = cs[cur], cs[1 - cur]
            k.op(dve, lambda: nc.vector.tensor_copy(out=b_[:], in_=a_[:]), W=[b_g])
            k.op(dve, lambda: nc.vector.tensor_tensor(out=b_[:, dsh:64], in0=a_[:, dsh:64], in1=a_[:, 0:64 - dsh], op=ALU.add), W=[b_g])
            cur = 1 - cur
        bend = cs[cur]
        k.op(dve, lambda: nc.vector.tensor_tensor(out=pst[:], in0=bend[:], in1=nbk[:], op=ALU.subtract), W=[b_g])
        k.op(dve, lambda: nc.vector.tensor_scalar(out=pst[:], in0=pst[:], scalar1=128.0, scalar2=None, op0=ALU.mult), W=[b_g])
        k.op(dve, lambda: nc.vector.tensor_tensor(out=cmp3[:], in0=bend[:].unsqueeze(1).broadcast_to([128, NBLK, 64]),
                                                  in1=iot[:, 0:NBLK].unsqueeze(2).broadcast_to([128, NBLK, 64]), op=ALU.is_le),
             R=[b_c2], W=[b_g])
        k.op(dve, lambda: nc.vector.tensor_reduce(out=bef[:], in_=cmp3[:], axis=AX.X, op=ALU.add), W=[b_g])
        k.op(dve, lambda: nc.vector.scalar_tensor_tensor(out=tf1[:, :, 0:8], in0=bef[:].unsqueeze(2).broadcast_to([128, NBLK, 8]), scalar=1024.0,
                                                         in1=iot[:, 96:104].unsqueeze(1).broadcast_to([128, NBLK, 8]),
                                                         op0=ALU.mult, op1=ALU.add), R=[b_c2], W=[b_g])
        k.op(dve, lambda: nc.vector.tensor_copy(out=idx_gu[:], in_=tf1[:, :, 0:8]), W=[b_g])
        k.op(dve, lambda: nc.vector.scalar_tensor_tensor(out=tf1[:, :, 0:4], in0=bef[:].unsqueeze(2).broadcast_to([128, NBLK, 4]), scalar=512.0,
                                                         in1=iot[:, 96:100].unsqueeze(1).broadcast_to([128, NBLK, 4]),
                                                         op0=ALU.mult, op1=ALU.add), R=[b_c2], W=[b_g])
        k.op(dve, lambda: nc.vector.tensor_copy(out=idx_dn[:], in_=tf1[:, :, 0:4]), W=[b_g])
        for s in range(16):
            pbank = 1 + s // 8
            col = pbank * 512 + (s % 8) * 64

            def mmRank(s=s, col=col):
                ins = nc.tensor.matmul(psall[:, col: col + 64], lhsT=utri[:], rhs=O12all[:, s, :],
                                       start=True, stop=(s == 0))
                for s2 in range(s):
                    ins = nc.tensor.matmul(psall[:, col: col + 64], lhsT=ones_bf[:], rhs=O12all[:, s2, :],
                                           start=False, stop=(s2 == s - 1))
                return ins
            k.op(pe, mmRank, R=[b_rt, b_c2], W=[psb[pbank]])
        prA = A.alloc("prA", [128, 16, 64])
        prB = A.alloc("prB", [128, 16, 64])
        k.op(dve, lambda: nc.vector.tensor_tensor(out=prA[:], in0=psall[:, 512:1536].rearrange("p (t e) -> p t e", e=64),
                                                  in1=pst[:].unsqueeze(1).broadcast_to([128, 16, 64]), op=ALU.add),
             R=[psb[1], psb[2]], W=[b_g])
        k.op(dve, lambda: nc.vector.tensor_tensor(out=prB[:], in0=prA[:], in1=O1all[:], op=ALU.mult), R=[b_rt], W=[b_g])
        k.op(dve, lambda: nc.vector.tensor_reduce(out=destf[:, :, 0], in_=prB[:], axis=AX.X, op=ALU.add), W=[b_g])
        k.op(dve, lambda: nc.vector.tensor_tensor(out=prB[:], in0=prA[:], in1=O2all[:], op=ALU.mult), R=[b_rt], W=[b_g])
        k.op(dve, lambda: nc.vector.tensor_reduce(out=destf[:, :, 1], in_=prB[:], axis=AX.X, op=ALU.add), W=[b_g])
        k.op(dve, lambda: nc.vector.tensor_copy(out=desti[:], in_=destf[:]), W=[b_g])
        if debug:
            for s in range(16):
                k.dma(k.q_sp, dbg_rt[:, s * 132: s * 132 + 64], O1all[:, s, :], R=[b_rt])
                k.dma(k.q_sp, dbg_rt[:, s * 132 + 64: s * 132 + 128], O2all[:, s, :], R=[b_rt])
                k.dma(k.q_sp, dbg_rt[:, s * 132 + 128: s * 132 + 130], w12all[:, s, :], R=[b_rt])
                k.dma(k.q_sp, dbg_rt[:, s * 132 + 130: s * 132 + 132], destf[:, s, :], R=[b_g])
            k.dma(k.q_sp, dbg_g[:, 0:64], cnt[:], R=[b_g])
            k.dma(k.q_sp, dbg_g[:, 64:160], bef[:], R=[b_g])
        k.barrier()
        if debug == 'G':
            return nc

        IOA = bass.IndirectOffsetOnAxis
        reg_gu = nc.gpsimd.alloc_register("bc_gu")
        nc.gpsimd.reg_mov(reg_gu, NE * 1024 - 1)
        reg_dn = nc.gpsimd.alloc_register("bc_dn")
        nc.gpsimd.reg_mov(reg_dn, NE * FF - 1)
        A.ptr = H_KEEP
        NWB = 3
        wgu = [A.alloc(f"wgu{i}", [128, KC, 2 * FF], BF16) for i in range(NWB)]
        wdn = [A.alloc(f"wdn{i}", [128, 4, D], BF16) for i in range(NWB)]
        xin = [A.alloc(f"xin{i}", [128, D], BF16) for i in range(2)]
        xinT = [A.alloc(f"xinT{i}", [128, KC, 128], BF16) for i in range(2)]
        sgt = [A.alloc(f"sgt{i}", [128, 512]) for i in range(2)]
        hmT = [A.alloc(f"hmT{i}", [128, 4, 128], BF16) for i in range(2)]
        yst = [A.alloc(f"yst{i}", [128, D]) for i in range(2)]
        b_wgu, b_wdn, b_xin, b_xinT, b_sgt, b_hmT, b_yst = [[Buf(), Buf(), Buf()] for _ in range(7)]
        for t in zero_tks:
            pool.wait(t)
        sc_tks = []
        for s in range(16):
            i = s % 2
            k.dma(k.q_sp, xin[i][:], h2_d[s * 128:(s + 1) * 128, :], W=[b_xin[i]])
            for j in range(2):
                sc_tks.append(k.dma(k.q_pool, xin_d[:, :], xin[i][:], R=[b_xin[i], b_g],
                                    indirect=dict(out_offset=IOA(ap=desti[:, s, j:j + 1].bitcast(U32), axis=0), in_offset=None)))
        for t in sc_tks:
            sp.wait(t)
        ys_tks = []
        border, wmap = [], []
        for j in range(NBLK // 4):
            border += [3 * j, 3 * j + 1, 3 * j + 2, NBLK - 1 - j]
            wmap += [(3 * j) % 2, (3 * j + 1) % 2, (3 * j + 2) % 2, 2]
        def h_loads(pos):
            b = border[pos]
            i = pos % 2
            wi = wmap[pos]
            k.dma(k.q_sp, xin[i][:], xin_d[b * 128:(b + 1) * 128, :], W=[b_xin[i]])
            for j in range(8):
                k.dma(k.q_pool, wgu[wi][:, 2 * j:2 * j + 2, :].rearrange("p c n -> p (c n)"), w_gu_d[:, :], R=[b_g], W=[b_wgu[wi]],
                      indirect=dict(out_offset=None, in_offset=IOA(ap=idx_gu[:, b, j:j + 1].bitcast(U32), axis=0),
                                    bounds_check=reg_gu, oob_is_err=False))
            for kc in range(4):
                k.dma(k.q_pool, wdn[wi][:, kc, :], w_dn_d[:, :], R=[b_g], W=[b_wdn[wi]],
                      indirect=dict(out_offset=None, in_offset=IOA(ap=idx_dn[:, b, kc:kc + 1].bitcast(U32), axis=0),
                                    bounds_check=reg_dn, oob_is_err=False))

        def h_front(pos):
            i = pos % 2
            for hf in range(2):
                pv = bank_bf(hf)

                def trX(hf=hf, pv=pv, i=i):
                    ins = None
                    for j in range(8):
                        c = hf * 8 + j
                        ins = nc.tensor.transpose(pv[:, j * 128:(j + 1) * 128], xin[i][:, c * 128:(c + 1) * 128], ident_bf[:])
                    return ins
                k.op(pe, trX, R=[b_xin[i], b_const], W=[psb[hf]])
                dstv = xinT[i][:, hf * 8:(hf + 1) * 8, :].rearrange("p c t -> p (c t)")
                if hf == 0:
                    k.op(act, lambda: nc.scalar.copy(out=dstv, in_=pv), R=[psb[hf]], W=[b_xinT[i]])
                else:
                    k.op(dve, lambda: nc.vector.tensor_copy(out=dstv, in_=pv), R=[psb[hf]], W=[b_xinT[i]])

        def h_mid(pos):
            i = pos % 2
            wi = wmap[pos]
            for gu in range(2):
                def mmGU(gu=gu, i=i, wi=wi):
                    ins = None
                    for m4 in range(4):
                        m = gu * 4 + m4
                        for kc in range(KC):
                            ins = nc.tensor.matmul(psall[:, (2 + gu) * 512 + m4 * 128: (2 + gu) * 512 + (m4 + 1) * 128],
                                                   lhsT=wgu[wi][:, kc, m * 128:(m + 1) * 128], rhs=xinT[i][:, kc, :],
                                                   start=(kc == 0), stop=(kc == KC - 1))
                    return ins
                k.op(pe, mmGU, R=[b_wgu[wi], b_xinT[i]], W=[psb[2 + gu]])
            k.op(act, lambda: nc.scalar.activation(out=sgt[i][:], in_=bank(2), func=AF.Silu), R=[psb[2]], W=[b_sgt[i]])
            k.op(dve, lambda: nc.vector.tensor_tensor(out=hmT[i][:].rearrange("p c t -> p (c t)"), in0=bank(3), in1=sgt[i][:], op=ALU.mult),
                 R=[psb[3], b_sgt[i]], W=[b_hmT[i]])

        def h_back(pos):
            b = border[pos]
            i = pos % 2
            wi = wmap[pos]
            for n in range(4):
                def mmD(n=n, i=i, wi=wi):
                    ins = None
                    for kc in range(4):
                        ins = nc.tensor.matmul(bank(4 + n), lhsT=hmT[i][:, kc, :], rhs=wdn[wi][:, kc, n * 512:(n + 1) * 512],
                                               start=(kc == 0), stop=(kc == 3))
                    return ins
                k.op(pe, mmD, R=[b_hmT[i], b_wdn[wi]], W=[psb[4 + n]])
                if n % 2 == 0:
                    k.op(act, lambda: nc.scalar.copy(out=yst[i][:, n * 512:(n + 1) * 512], in_=bank(4 + n)), R=[psb[4 + n]], W=[b_yst[i]])
                else:
                    k.op(dve, lambda: nc.vector.tensor_copy(out=yst[i][:, n * 512:(n + 1) * 512], in_=bank(4 + n)), R=[psb[4 + n]], W=[b_yst[i]])
            ys_tks.append(k.dma(k.q_sp, ys_d[b * 128:(b + 1) * 128, :], yst[i][:], R=[b_yst[i]]))

        h_loads(0)
        h_front(0)
        for pos in range(NBLK):
            if pos + 1 < NBLK:
                h_loads(pos + 1)
            h_mid(pos)
            if pos + 1 < NBLK:
                h_front(pos + 1)
            h_back(pos)
        k.barrier()

        A.ptr = H_KEEP
        g2bc = A.alloc("g2bc", [128, D])
        gfbc = A.alloc("gfbc", [128, D])
        b_c3 = Buf()
        k.dma(k.q_sp, g2bc[:], modvec[4].partition_broadcast(128), W=[b_c3])
        k.dma(k.q_sp, gfbc[:], gfin_d.partition_broadcast(128), W=[b_c3])
        x1c = [A.alloc(f"x1c{i}", [128, D]) for i in range(2)]
        y1 = [A.alloc(f"y1_{i}", [128, D]) for i in range(2)]
        y2 = [A.alloc(f"y2_{i}", [128, D]) for i in range(2)]
        ot = [A.alloc(f"ot{i}", [128, D]) for i in range(2)]
        junk3 = A.alloc("junk3", [128, D], BF16)
        ss3 = [A.alloc(f"ss3_{i}", [128, 1]) for i in range(2)]
        b_x1c, b_y1, b_y2, b_ot, b_ss3 = [[Buf(), Buf()] for _ in range(5)]
        b_junk3 = Buf()
        def loadsI(s):
            i = s % 2
            k.dma(k.q_sp, x1c[i][:], x1_d[s * 128:(s + 1) * 128, :], W=[b_x1c[i]])
            k.dma(k.q_pool, y1[i][:], ys_d[:, :], R=[b_g], W=[b_y1[i]],
                  indirect=dict(out_offset=None, in_offset=IOA(ap=desti[:, s, 0:1].bitcast(U32), axis=0)))
            k.dma(k.q_pool, y2[i][:], ys_d[:, :], R=[b_g], W=[b_y2[i]],
                  indirect=dict(out_offset=None, in_offset=IOA(ap=desti[:, s, 1:2].bitcast(U32), axis=0)))

        def computeI(s):
            i = s % 2
            k.op(act, lambda: nc.scalar.activation(out=y1[i][:], in_=y1[i][:], func=AF.Copy, scale=w12all[:, s, 0:1]),
                 R=[b_rt], W=[b_y1[i]])
            k.op(dve, lambda: nc.vector.scalar_tensor_tensor(out=y1[i][:], in0=y2[i][:], scalar=w12all[:, s, 1:2], in1=y1[i][:],
                                                             op0=ALU.mult, op1=ALU.add),
                 R=[b_y2[i], b_rt], W=[b_y1[i]])
            k.op(pool, lambda: nc.gpsimd.tensor_tensor(out=y1[i][:], in0=y1[i][:], in1=g2bc[:], op=ALU.mult), R=[b_c3], W=[b_y1[i]])
            k.op(pool, lambda: nc.gpsimd.tensor_tensor(out=x1c[i][:], in0=x1c[i][:], in1=y1[i][:], op=ALU.add), R=[b_y1[i]], W=[b_x1c[i]])
            k.op(act, lambda: nc.scalar.activation(out=junk3[:], in_=x1c[i][:], func=AF.Square, accum_out=ss3[i][:]),
                 R=[b_x1c[i]], W=[b_junk3, b_ss3[i]])
            k.op(act, lambda: nc.scalar.activation(out=ss3[i][:], in_=ss3[i][:], func=AF.Sqrt, scale=1.0 / D, bias=1e-6), W=[b_ss3[i]])
            k.op(dve, lambda: nc.vector.reciprocal(out=ss3[i][:], in_=ss3[i][:]), W=[b_ss3[i]])
            k.op(dve, lambda: nc.vector.scalar_tensor_tensor(out=ot[i][:], in0=x1c[i][:], scalar=ss3[i][:, 0:1], in1=gfbc[:],
                                                             op0=ALU.mult, op1=ALU.mult),
                 R=[b_x1c[i], b_ss3[i], b_c3], W=[b_ot[i]])
            k.dma(k.q_sp, out[s * 128:(s + 1) * 128, :], ot[i][:], R=[b_ot[i]])

        loadsI(0)
        for s in range(16):
            if s + 1 < 16:
                loadsI(s + 1)
            computeI(s)
        k.barrier()
    return nc


_CACHE = {}


def _w_in_cols():
    cols = []
    qcols = np.arange(1024)
    hl = qcols % 64
    qswap = qcols - hl + (hl + 32) % 64
    for c in range(8):
        cols.append(qcols[c * 128:(c + 1) * 128])
    for c in range(8):
        cols.append(qswap[c * 128:(c + 1) * 128])
    for g in range(2):
        kc_ = 1024 + g * 64 + np.arange(64)
        ks_ = 1024 + g * 64 + (np.arange(64) + 32) % 64
        cols.append(np.concatenate([kc_, kc_]))
        cols.append(np.concatenate([ks_, ks_]))
    for c in range(8):
        cols.append(1280 + np.arange(c * 128, (c + 1) * 128))
    for c in range(8):
        cols.append(2304 + np.arange(c * 128, (c + 1) * 128))
    cols.append(1152 + np.arange(128))
    return cols


def _host_inputs(inp):
    f32 = np.float32
    x = np.ascontiguousarray(inp["x"], dtype=f32)
    c = np.asarray(inp["c"], dtype=f32)
    pos = np.asarray(inp["positions"]).astype(np.int32)
    b_adaT = np.ascontiguousarray(np.asarray(inp["b_ada"], f32).reshape(96, 128).T)
    gvecT = np.ascontiguousarray(np.concatenate([
        np.asarray(inp["g_mix"], f32).reshape(KC, 128).T,
        np.asarray(inp["g_ffn"], f32).reshape(KC, 128).T,
        np.asarray(inp["b_out"], f32).reshape(KC, 128).T], axis=1))
    w_ada = np.ascontiguousarray(inp["w_ada"], dtype=f32)
    identf = np.eye(128, dtype=f32)
    cols = _w_in_cols()
    allc = np.concatenate(cols)
    w_in = np.asarray(inp["w_in"], f32)
    b_in = np.asarray(inp["b_in"], f32)
    wp = w_in[:, allc].reshape(KC, 128, 37, 128)
    w_in_c = np.ascontiguousarray(wp.transpose(2, 1, 0, 3).reshape(37, 128, D))
    b_inT = np.ascontiguousarray(b_in[allc[:36 * 128]].reshape(36, 128).T)
    bv = np.ascontiguousarray(b_in[1152:1280])
    p = np.arange(128)
    invf32 = (f32(10000.0) ** (-np.arange(32, dtype=f32) * f32(2.0) / f32(64))).astype(f32)
    q_ = np.arange(128)[:, None]
    j_ = np.arange(256)[None, :]
    valid = (j_ >= q_ + 1) & (j_ <= q_ + 128)
    mask_std = np.where(valid, 0.0, -1e30).astype(f32)
    mask_first = mask_std.copy()
    mask_first[:, :128] = -1e30
    conv_w = np.asarray(inp["conv_w"], f32)
    cw = np.ascontiguousarray(conv_w.T.reshape(8, 128, CSZ).transpose(1, 0, 2).reshape(128, 8 * CSZ))
    cvec = np.ascontiguousarray(np.concatenate([
        np.asarray(inp["conv_b"], f32).reshape(8, 128).T,
        np.asarray(inp["conv_ln_g"], f32).reshape(8, 128).T,
        np.asarray(inp["conv_ln_b"], f32).reshape(8, 128).T], axis=1))
    w_out = np.ascontiguousarray(inp["w_out"], dtype=f32)
    sinks = np.ascontiguousarray(np.asarray(inp["attn_sinks"], f32).reshape(4, 2, 2).transpose(0, 2, 1).reshape(16))
    w_rf = np.concatenate([np.asarray(inp["w_group_router"], f32), np.asarray(inp["w_expert_router"], f32)], axis=1)
    w_r = np.ascontiguousarray(w_rf.reshape(KC, 128, 72).transpose(1, 0, 2).reshape(128, KC * 72))
    b_r = np.ascontiguousarray(np.concatenate([np.asarray(inp["b_group_router"], f32), np.asarray(inp["b_expert_router"], f32)]))
    g_final = np.ascontiguousarray(inp["g_final"], dtype=f32)
    iotas = np.zeros((128, 96 + 16 + 4), f32)
    iotas[:, 0:96] = np.arange(96, dtype=f32)[None, :]
    iotas[:, 96:112] = (np.arange(16)[None, :] * 128 + p[:, None]).astype(f32)
    iotas[:, 112:116] = (np.arange(4)[None, :] * 128 + p[:, None]).astype(f32)
    utri = (p[:, None] < p[None, :]).astype(f32)
    w_gu = np.ascontiguousarray(np.asarray(inp["w_gate_up"], f32).reshape(NE, 8, 2, 128, 2 * FF).transpose(0, 1, 3, 2, 4)
                                ).reshape(NE * 8 * 128, 2 * 2 * FF)
    w_dn = np.ascontiguousarray(inp["w_down"], dtype=f32).reshape(NE * 4 * 128, D)
    maps = []
    for core in range(NCORES):
        b = core // 4
        s0 = (core % 4) * NT
        first = (s0 == 0)
        xh = np.zeros((NTT, D), f32)
        ph = np.zeros((NTT,), np.int32)
        if not first:
            xh[:NH] = x[b, s0 - NH:s0]
            ph[:NH] = pos[b, s0 - NH:s0]
        xh[NH:] = x[b, s0:s0 + NT]
        ph[NH:] = pos[b, s0:s0 + NT]
        smallc = np.zeros((128, 4), f32)
        smallc[:, 0] = invf32[p % 32]
        smallc[:, 1] = np.where(p % 64 < 32, -1.0, 1.0)
        smallc[:, 2] = 0.0 if first else 1.0
        masks = np.stack([mask_std, mask_first if first else mask_std], 0)
        m = {
            "xh": xh,
            "cT": np.ascontiguousarray(c[b].reshape(KC, 128).T),
            "w_ada": w_ada,
            "b_adaT": b_adaT,
            "gvecT": gvecT,
            "identf": identf,
            "w_in_c": w_in_c,
            "b_inT": b_inT,
            "bv": bv,
            "pos": ph,
            "smallc": smallc,
            "masks": np.ascontiguousarray(masks),
            "sinks": sinks,
            "cw": cw,
            "cvec": cvec,
            "w_out": w_out,
            "w_r": w_r,
            "b_r": b_r,
            "g_final": g_final,
            "iotas": iotas,
            "utri": utri,
            "w_gu": w_gu,
            "w_dn": w_dn,
        }
        maps.append(m)
    return maps


def kernel(**inputs):
    if "nc" not in _CACHE:
        _CACHE["nc"] = build_program()
    nc = _CACHE["nc"]
    maps = _host_inputs(inputs)
    res = run_bass_kernel_spmd(nc, maps, core_ids=list(range(NCORES)))
    outs = [np.asarray(r["out"]) for r in res.results]
    y = np.stack(outs, 0).reshape(2, 4 * NT, D).astype(np.float32)
    return y
```
